# Optimizing a Trainium2 kernel written in Bass

```python
import math
import jax
import jax.numpy as jnp
from jax import lax
import numpy as np

D_MODEL = 1024
BATCH = 8
SEQ = 4096
DEPTH = 2

CTX_LEN = 256
GRID_W = 64
EPS = 1e-6
NEG_INF = -1e30

RET_HEADS = 4
RET_DK = 128
RET_DV = 128
RET_CHUNK = 128
RET_ROPE_BASE = 10000.0
ATT_HEADS = 8
ATT_KV_HEADS = 2
ATT_HEAD_DIM = 64
ATT_WINDOW = 128
ATT_BLOCK = 128
ROPE_BASE = 10000.0
HY_WIDTH = 512
HY_SHORT = 3
HY_BANDS = 16
HY_EMB = 1 + 2 * HY_BANDS
HY_FFN = 64
HY_SLOW_DECAY_PCT = 1.5
HY_FAST_DECAY_PCT = 0.3
HY_DECAY_TARGET = 1e-2
N_BRANCH = 3
BRANCH_WIDTH = 512
N_EXPERTS = 32
TOP_K = 4
D_FF = 1024
SWIGLU_ALPHA = 1.702
SWIGLU_LIMIT = 7.0

RET_W = RET_HEADS * RET_DK
RET_VW = RET_HEADS * RET_DV
ATT_QW = ATT_HEADS * ATT_HEAD_DIM
ATT_KW = ATT_KV_HEADS * ATT_HEAD_DIM
HY_IN = 3 * HY_WIDTH
GATE_W = N_BRANCH * D_MODEL
IN_COLS = 2 * RET_W + 2 * RET_VW + ATT_QW + 2 * ATT_KW + HY_IN + GATE_W
IN_SPLITS = (RET_W,
             2 * RET_W,
             2 * RET_W + RET_VW,
             2 * RET_W + 2 * RET_VW,
             2 * RET_W + 2 * RET_VW + ATT_QW,
             2 * RET_W + 2 * RET_VW + ATT_QW + ATT_KW,
             2 * RET_W + 2 * RET_VW + ATT_QW + 2 * ATT_KW,
             2 * RET_W + 2 * RET_VW + ATT_QW + 2 * ATT_KW + HY_IN)

kernel_name = "hybrid_parallel_mixers_moe_dit_trunk"


def _rms_norm(x, g):
    xf = x.astype(jnp.float32)
    xf = xf * lax.rsqrt(jnp.mean(xf * xf, axis=-1, keepdims=True) + EPS)
    return xf.astype(x.dtype) * g


def _modulate(x, g, shift, scale):
    return _rms_norm(x, g) * (1.0 + scale) + shift


def _rope(x, pos, inv_freq):
    n = x.shape[-1] // 2
    ang = pos[:, None] * inv_freq[None, :]
    cos = jnp.cos(ang)[None, :, None, :]
    sin = jnp.sin(ang)[None, :, None, :]
    x1 = x[..., :n].astype(jnp.float32)
    x2 = x[..., n:].astype(jnp.float32)
    return jnp.concatenate([x1 * cos - x2 * sin, x1 * sin + x2 * cos], axis=-1).astype(x.dtype)


def _rope_axial(x, rows, cols):
    half = x.shape[-1] // 2
    nf = half // 2
    inv = 1.0 / (ROPE_BASE ** (jnp.arange(nf, dtype=jnp.float32) / nf))
    return jnp.concatenate([_rope(x[..., :half], rows, inv), _rope(x[..., half:], cols, inv)], axis=-1)


def _retention_dir(q, k, v, log_g, s0, inclusive):
    B, L, H, dk = q.shape
    dv = v.shape[-1]
    C = RET_CHUNK
    n = L // C
    qc = q.reshape(B, n, C, H, dk)
    kc = k.reshape(B, n, C, H, dk)
    vc = v.reshape(B, n, C, H, dv)
    idx = jnp.arange(C, dtype=jnp.float32)
    diff = idx[:, None] - idx[None, :]
    keep = (diff >= 0) if inclusive else (diff > 0)
    dmat = jnp.where(keep[None], jnp.exp(log_g[:, None, None] * jnp.maximum(diff, 0.0)[None]), 0.0)
    scores = jnp.einsum('bnihd,bnjhd->bnhij', qc, kc) * dmat
    inner = jnp.einsum('bnhij,bnjhe->bnihe', scores, vc)
    w_state = jnp.exp(log_g[None, :] * (C - 1.0 - idx)[:, None])
    kv = jnp.einsum('bnjhd,jh,bnjhe->nbhde', kc, w_state, vc)
    chunk_decay = jnp.exp(log_g * C)[None, :, None, None]

    def step(s, kv_c):
        return chunk_decay * s + kv_c, s

    s_fin, s_prev = lax.scan(step, s0, kv)
    w_read = jnp.exp(log_g[None, :] * (idx + 1.0)[:, None])
    cross = jnp.einsum('bnihd,ih,nbhde->bnihe', qc, w_read, s_prev)
    return (inner + cross).reshape(B, L, H, dv), s_fin


def _retention_bidir(q, k, v, log_g, s0_fwd, s0_bwd):
    y_f, s_f = _retention_dir(q, k, v, log_g[0], s0_fwd, True)
    y_b, s_b = _retention_dir(q[:, ::-1], k[:, ::-1], v[:, ::-1], log_g[1], s0_bwd, False)
    return y_f + y_b[:, ::-1], s_f, s_b


def _head_rms(y):
    y = y * lax.rsqrt(jnp.mean(y * y, axis=-1, keepdims=True) + EPS)
    return y.reshape(y.shape[0], y.shape[1], -1)


def _softmax_with_sink(s, sink):
    G = sink.shape[0] // ATT_KV_HEADS
    sk = sink.astype(jnp.float32).reshape(1, ATT_KV_HEADS, G, 1, 1)
    m = jnp.maximum(jnp.max(s, axis=-1, keepdims=True), sk)
    e = jnp.exp(s - m)
    return e / (jnp.sum(e, axis=-1, keepdims=True) + jnp.exp(sk - m))


def _attn_latent(q, k, v, k_ctx, v_ctx, sink):
    B, L, H, d = q.shape
    G = H // ATT_KV_HEADS
    nb = L // ATT_BLOCK
    span = ATT_BLOCK + 2 * ATT_WINDOW
    Lc = k_ctx.shape[1]
    qg = q.reshape(B, L, ATT_KV_HEADS, G, d) * (d ** -0.5)
    pad = ((0, 0), (ATT_WINDOW, ATT_WINDOW), (0, 0), (0, 0))
    k_pad = jnp.pad(k, pad)
    v_pad = jnp.pad(v, pad)
    ctx_valid = jnp.ones((ATT_BLOCK, Lc), dtype=bool)

    def one_block(bi):
        start = bi * ATT_BLOCK
        qb = lax.dynamic_slice_in_dim(qg, start, ATT_BLOCK, axis=1)
        kb = jnp.concatenate([lax.dynamic_slice_in_dim(k_pad, start, span, axis=1), k_ctx], axis=1)
        vb = jnp.concatenate([lax.dynamic_slice_in_dim(v_pad, start, span, axis=1), v_ctx], axis=1)
        s = jnp.einsum('bqhgd,bkhd->bhgqk', qb, kb).astype(jnp.float32)
        qpos = start + jnp.arange(ATT_BLOCK)
        kpos = start - ATT_WINDOW + jnp.arange(span)
        valid = ((kpos[None, :] >= 0) & (kpos[None, :] < L)
                 & (jnp.abs(qpos[:, None] - kpos[None, :]) <= ATT_WINDOW))
        valid = jnp.concatenate([valid, ctx_valid], axis=1)
        p = _softmax_with_sink(jnp.where(valid, s, NEG_INF), sink)
        o = jnp.einsum('bhgqk,bkhd->bqhgd', p.astype(vb.dtype), vb)
        return o.reshape(B, ATT_BLOCK, H * d)

    out = lax.map(one_block, jnp.arange(nb))
    return jnp.transpose(out, (1, 0, 2, 3)).reshape(B, L, H * d)


def _attn_context(q, k, v, sink):
    B, Lc, H, d = q.shape
    G = H // ATT_KV_HEADS
    qg = q.reshape(B, Lc, ATT_KV_HEADS, G, d) * (d ** -0.5)
    s = jnp.einsum('bqhgd,bkhd->bhgqk', qg, k).astype(jnp.float32)
    p = _softmax_with_sink(s, sink)
    o = jnp.einsum('bhgqk,bkhd->bqhgd', p.astype(v.dtype), v)
    return o.reshape(B, Lc, H * d)


def _short_conv(u, w, b):
    L = u.shape[1]
    p = HY_SHORT // 2
    up = jnp.pad(u, ((0, 0), (p, HY_SHORT - 1 - p), (0, 0)))
    y = b
    for j in range(HY_SHORT):
        y = y + up[:, j:j + L] * w[j]
    return y


def _hyena_filter(L, w1, b1, f1, w2, b2, f2, w3):
    f32 = jnp.float32
    t = jnp.linspace(0.0, 1.0, L, dtype=f32)[:, None]
    bands = jnp.linspace(1e-4, HY_BANDS - 1, HY_BANDS, dtype=f32)
    ang = (2.0 * math.pi / L) * jnp.arange(L, dtype=f32)[:, None] * bands[None, :]
    z = jnp.concatenate([t, jnp.cos(ang), -jnp.sin(ang)], axis=-1)
    h = jnp.sin(f1.astype(f32) * (z @ w1.astype(f32) + b1.astype(f32)))
    h = jnp.sin(f2.astype(f32) * (h @ w2.astype(f32) + b2.astype(f32)))
    h = h @ w3.astype(f32)
    deltas = jnp.abs(jnp.linspace(math.log(HY_DECAY_TARGET) / HY_SLOW_DECAY_PCT,
                                  math.log(HY_DECAY_TARGET) / HY_FAST_DECAY_PCT, HY_WIDTH, dtype=f32))
    decay = jnp.exp(-t * deltas[None, :])
    h_fwd = h[:, :HY_WIDTH] * decay
    h_bwd = h[:, HY_WIDTH:] * decay
    l1 = jnp.sum(jnp.abs(h_fwd), axis=0) + jnp.sum(jnp.abs(h_bwd[1:]), axis=0)
    h_fwd = h_fwd / l1
    h_bwd = h_bwd / l1
    filt2l = jnp.concatenate([h_fwd, jnp.zeros((1, HY_WIDTH), f32), h_bwd[:0:-1]], axis=0)
    return jnp.fft.rfft(filt2l, axis=0)


def _long_conv(u, filt_f):
    L = u.shape[1]
    uf = jnp.fft.rfft(u.astype(jnp.float32), n=2 * L, axis=1)
    y = jnp.fft.irfft(uf * filt_f[None], n=2 * L, axis=1)[:, :L]
    return y.astype(u.dtype)


def _hyena_seq(u, conv_w, conv_b, skip, filt_params):
    L = u.shape[1]
    u = _short_conv(u, conv_w, conv_b)
    x0, x1, v = jnp.split(u, 3, axis=-1)
    z = x1 * v
    z = _long_conv(z, _hyena_filter(L, *filt_params)) + skip * z
    return x0 * z


def _merge(ret, att, hy, gate_cols, w_branch, b_gate, w_out):
    g = jax.nn.sigmoid(gate_cols + b_gate)
    g_r, g_a, g_h = jnp.split(g, N_BRANCH, axis=-1)
    m = g_r * (ret @ w_branch[0]) + g_a * (att @ w_branch[1]) + g_h * (hy @ w_branch[2])
    return m @ w_out


def _moe(h, router_w, router_b, w1, b1, w2, b2):
    shp = h.shape
    t = h.reshape(-1, shp[-1])
    logits = (t @ router_w + router_b).astype(jnp.float32)
    top_v, top_i = lax.top_k(logits, TOP_K)
    wts = jax.nn.softmax(top_v, axis=-1)
    combine = jnp.sum(jax.nn.one_hot(top_i, N_EXPERTS, dtype=jnp.float32) * wts[..., None], axis=1).astype(t.dtype)
    y = jnp.zeros_like(t)
    for e in range(N_EXPERTS):
        hh = t @ w1[e] + b1[e]
        glu = jnp.minimum(hh[:, :D_FF], SWIGLU_LIMIT)
        lin = jnp.clip(hh[:, D_FF:], -SWIGLU_LIMIT, SWIGLU_LIMIT)
        act = glu * jax.nn.sigmoid(SWIGLU_ALPHA * glu) * (lin + 1.0)
        y = y + combine[:, e:e + 1] * (act @ w2[e] + b2[e])
    return y.reshape(shp)


def _layer(x, ctx, c, c_ctx, w_mod, b_mod, norm1_g, w_in, ret_decay_logit, attn_sink,
           hy_conv_w, hy_conv_b, hy_filter, hy_skip, w_branch, b_gate, w_out, norm2_g,
           router_w, router_b, moe_w1, moe_b1, moe_w2, moe_b2, tpos, rows, cols, last):
    B, L, _ = x.shape
    Lc = ctx.shape[1]
    mod_l = (jax.nn.silu(c) @ w_mod + b_mod)[:, None, :]
    mod_c = (jax.nn.silu(c_ctx) @ w_mod + b_mod)[None, None, :]
    sh1_l, sc1_l, g1_l, sh2_l, sc2_l, g2_l = jnp.split(mod_l, 6, axis=-1)
    sh1_c, sc1_c, g1_c, sh2_c, sc2_c, g2_c = jnp.split(mod_c, 6, axis=-1)

    pl = _modulate(x, norm1_g, sh1_l, sc1_l) @ w_in
    pc = _modulate(ctx, norm1_g, sh1_c, sc1_c) @ w_in
    rq_l, rk_l, rv_l, rg_l, aq_l, ak_l, av_l, hu_l, mg_l = jnp.split(pl, IN_SPLITS, axis=-1)
    rq_c, rk_c, rv_c, rg_c, aq_c, ak_c, av_c, hu_c, mg_c = jnp.split(pc, IN_SPLITS, axis=-1)

    f32 = jnp.float32
    log_g = jax.nn.log_sigmoid(ret_decay_logit.astype(f32))
    inv_r = 1.0 / (RET_ROPE_BASE ** jnp.linspace(0.0, 1.0, RET_DK // 2, dtype=f32))
    k_scale = RET_DK ** -0.5
    q_rl = _rope(rq_l.reshape(B, L, RET_HEADS, RET_DK), tpos, inv_r).astype(f32)
    k_rl = (_rope(rk_l.reshape(B, L, RET_HEADS, RET_DK), tpos, inv_r) * k_scale).astype(f32)
    v_rl = rv_l.reshape(B, L, RET_HEADS, RET_DV).astype(f32)
    q_rc = rq_c.reshape(B, Lc, RET_HEADS, RET_DK).astype(f32)
    k_rc = (rk_c.reshape(B, Lc, RET_HEADS, RET_DK) * k_scale).astype(f32)
    v_rc = rv_c.reshape(B, Lc, RET_HEADS, RET_DV).astype(f32)
    s0 = jnp.zeros((B, RET_HEADS, RET_DK, RET_DV), f32)
    y_rc, s_f, s_b = _retention_bidir(q_rc, k_rc, v_rc, log_g, s0, s0)
    y_rl, _, _ = _retention_bidir(q_rl, k_rl, v_rl, log_g, s_f, s_b)
    ret_l = _head_rms(y_rl).astype(x.dtype) * jax.nn.silu(rg_l)

    q_al = _rope_axial(aq_l.reshape(B, L, ATT_HEADS, ATT_HEAD_DIM), rows, cols)
    k_al = _rope_axial(ak_l.reshape(B, L, ATT_KV_HEADS, ATT_HEAD_DIM), rows, cols)
    v_al = av_l.reshape(B, L, ATT_KV_HEADS, ATT_HEAD_DIM)
    k_ac = ak_c.reshape(B, Lc, ATT_KV_HEADS, ATT_HEAD_DIM)
    v_ac = av_c.reshape(B, Lc, ATT_KV_HEADS, ATT_HEAD_DIM)
    att_l = _attn_latent(q_al, k_al, v_al, k_ac, v_ac, attn_sink)

    hy_l = _hyena_seq(hu_l, hy_conv_w, hy_conv_b, hy_skip, hy_filter)

    x_new = x + g1_l * _merge(ret_l, att_l, hy_l, mg_l, w_branch, b_gate, w_out)
    x_new = x_new + g2_l * _moe(_modulate(x_new, norm2_g, sh2_l, sc2_l),
                                router_w, router_b, moe_w1, moe_b1, moe_w2, moe_b2)
    if last:
        return x_new, ctx

    ret_c = _head_rms(y_rc).astype(ctx.dtype) * jax.nn.silu(rg_c)
    att_c = _attn_context(aq_c.reshape(B, Lc, ATT_HEADS, ATT_HEAD_DIM), k_ac, v_ac, attn_sink)
    hy_c = _hyena_seq(hu_c, hy_conv_w, hy_conv_b, hy_skip, hy_filter)
    ctx_new = ctx + g1_c * _merge(ret_c, att_c, hy_c, mg_c, w_branch, b_gate, w_out)
    ctx_new = ctx_new + g2_c * _moe(_modulate(ctx_new, norm2_g, sh2_c, sc2_c),
                                    router_w, router_b, moe_w1, moe_b1, moe_w2, moe_b2)
    return x_new, ctx_new


def setup_inputs(seed: int = 0) -> dict:
    key = jax.random.key(seed)
    ks = jax.random.split(key, 32)
    D = D_MODEL

    def nrm(k, shape, scale):
        return jax.random.normal(k, shape, dtype=jnp.float32) * scale

    gam = 1.0 - 2.0 ** (-5.0 - jnp.arange(RET_HEADS, dtype=jnp.float32))
    decay_logit0 = jnp.log(gam) - jnp.log1p(-gam)
    return {
        "x": nrm(ks[0], (BATCH, SEQ, D), 1.0),
        "c": nrm(ks[1], (BATCH, D), 1.0),
        "ctx": nrm(ks[2], (BATCH, CTX_LEN, D), 1.0),
        "c_ctx": nrm(ks[3], (D,), 1.0),
        "w_mod": nrm(ks[4], (DEPTH, D, 6 * D), 0.5 * D ** -0.5),
        "b_mod": nrm(ks[5], (DEPTH, 6 * D), 0.01),
        "norm1_g": 1.0 + nrm(ks[6], (DEPTH, D), 0.05),
        "w_in": nrm(ks[7], (DEPTH, D, IN_COLS), D ** -0.5),
        "ret_decay_logit": decay_logit0[None, None, :] + nrm(ks[8], (DEPTH, 2, RET_HEADS), 0.1),
        "attn_sink": nrm(ks[9], (DEPTH, ATT_HEADS), 0.5),
        "hy_conv_w": nrm(ks[10], (DEPTH, HY_SHORT, HY_IN), HY_SHORT ** -0.5),
        "hy_conv_b": nrm(ks[11], (DEPTH, HY_IN), 0.01),
        "hy_w1": nrm(ks[12], (DEPTH, HY_EMB, HY_FFN), HY_EMB ** -0.5),
        "hy_b1": nrm(ks[13], (DEPTH, HY_FFN), 0.1),
        "hy_freq1": 1.0 + nrm(ks[14], (DEPTH, HY_FFN), 0.05),
        "hy_w2": nrm(ks[15], (DEPTH, HY_FFN, HY_FFN), HY_FFN ** -0.5),
        "hy_b2": nrm(ks[16], (DEPTH, HY_FFN), 0.1),
        "hy_freq2": 1.0 + nrm(ks[17], (DEPTH, HY_FFN), 0.05),
        "hy_w3": nrm(ks[18], (DEPTH, HY_FFN, 2 * HY_WIDTH), HY_FFN ** -0.5),
        "hy_skip": nrm(ks[19], (DEPTH, HY_WIDTH), 0.5),
        "w_branch": nrm(ks[20], (DEPTH, N_BRANCH, BRANCH_WIDTH, D), BRANCH_WIDTH ** -0.5),
        "b_gate": nrm(ks[21], (DEPTH, GATE_W), 0.01),
        "w_out": nrm(ks[22], (DEPTH, D, D), D ** -0.5),
        "norm2_g": 1.0 + nrm(ks[23], (DEPTH, D), 0.05),
        "router_w": nrm(ks[24], (DEPTH, D, N_EXPERTS), D ** -0.5),
        "router_b": nrm(ks[25], (DEPTH, N_EXPERTS), 0.01),
        "moe_w1": nrm(ks[26], (DEPTH, N_EXPERTS, D, 2 * D_FF), D ** -0.5),
        "moe_b1": nrm(ks[27], (DEPTH, N_EXPERTS, 2 * D_FF), 0.01),
        "moe_w2": nrm(ks[28], (DEPTH, N_EXPERTS, D_FF, D), D_FF ** -0.5),
        "moe_b2": nrm(ks[29], (DEPTH, N_EXPERTS, D), 0.01),
        "final_norm_g": 1.0 + nrm(ks[30], (D,), 0.05),
    }


def reference(x, c, ctx, c_ctx, w_mod, b_mod, norm1_g, w_in, ret_decay_logit, attn_sink,
              hy_conv_w, hy_conv_b, hy_w1, hy_b1, hy_freq1, hy_w2, hy_b2, hy_freq2, hy_w3,
              hy_skip, w_branch, b_gate, w_out, norm2_g, router_w, router_b,
              moe_w1, moe_b1, moe_w2, moe_b2, final_norm_g):
    L = x.shape[1]
    ROWS = L // GRID_W
    tpos = jnp.arange(L, dtype=jnp.float32)
    rows = jnp.repeat(jnp.arange(ROWS, dtype=jnp.float32), GRID_W)
    cols = jnp.tile(jnp.arange(GRID_W, dtype=jnp.float32), ROWS)
    for l in range(DEPTH):
        hy_filter = (hy_w1[l], hy_b1[l], hy_freq1[l], hy_w2[l], hy_b2[l], hy_freq2[l], hy_w3[l])
        x, ctx = _layer(x, ctx, c, c_ctx, w_mod[l], b_mod[l], norm1_g[l], w_in[l],
                        ret_decay_logit[l], attn_sink[l], hy_conv_w[l], hy_conv_b[l],
                        hy_filter, hy_skip[l], w_branch[l], b_gate[l], w_out[l], norm2_g[l],
                        router_w[l], router_b[l], moe_w1[l], moe_b1[l], moe_w2[l], moe_b2[l],
                        tpos, rows, cols, l == DEPTH - 1)
    return _rms_norm(x, final_norm_g)
```

```python
import math
from contextlib import ExitStack

import numpy as np
import ml_dtypes
import concourse.bass as bass
import concourse.mybir as mybir
from concourse.bass_utils import run_bass_kernel_spmd

F32 = mybir.dt.float32
BF16 = mybir.dt.bfloat16
AF = mybir.ActivationFunctionType
ALU = mybir.AluOpType

D = 1024
L = 4096
LC = 256
NT = 34
NTOK = NT * 128
DEPTH = 2
EPS = 1e-6
IN_COLS = 7424
C_RQ, C_RK, C_RV, C_RG, C_AQ, C_AK, C_AV, C_HU, C_MG = 0, 512, 1024, 1536, 2048, 2560, 2688, 2816, 4352
NEXP = 32
DFF = 1024


class Truncate(Exception):
    pass


class Buf:
    __slots__ = ("w", "r", "x")

    def __init__(self, excl=False):
        self.w = {}
        self.r = {}
        self.x = excl


class Eng:
    def __init__(self, name, eng, sem, is_pe=False):
        self.name, self.eng, self.sem, self.is_pe = name, eng, sem, is_pe
        self.count = 0
        self.seen = {}


class FW:
    NDMA = 32

    def __init__(self, nc):
        self.nc = nc
        self.es = ExitStack()
        mk = lambda n: self.es.enter_context(nc.semaphore(n))
        self.pe = Eng("pe", nc.tensor, mk("s_pe"), True)
        self.act = Eng("act", nc.scalar, mk("s_act"))
        self.dve = Eng("dve", nc.vector, mk("s_dve"))
        self.pool = Eng("pool", nc.gpsimd, mk("s_pool"))
        self.sp = Eng("sp", nc.sync, mk("s_sp"))
        self.engs = [self.pe, self.act, self.dve, self.pool, self.sp]
        self.dsems = [mk(f"s_d{i}") for i in range(self.NDMA)]
        self.dval = [0] * self.NDMA
        self.dnext = 0
        self.dnext_sw = 0
        self.semof = {e.name: e.sem for e in self.engs}
        for i in range(self.NDMA):
            self.semof[("d", i)] = self.dsems[i]
        self.ninst = 0

    def _need(self, E, reads, writes):
        deps = {}
        for b in reads:
            for k, v in b.w.items():
                if deps.get(k, 0) < v:
                    deps[k] = v
            if b.x:
                for k, v in b.r.items():
                    if k != E.name and deps.get(k, 0) < v:
                        deps[k] = v
        for b in writes:
            for k, v in b.w.items():
                if deps.get(k, 0) < v:
                    deps[k] = v
            for k, v in b.r.items():
                if deps.get(k, 0) < v:
                    deps[k] = v
        for k, v in deps.items():
            if k == E.name and E.is_pe:
                continue
            if E.seen.get(k, 0) >= v:
                continue
            E.eng.wait_ge(self.semof[k], v)
            E.seen[k] = v

    max_ops = None

    def op(self, E, reads, writes, fn):
        if self.max_ops is not None and self.ninst >= self.max_ops:
            raise Truncate()
        self._need(E, reads, writes)
        ins = fn()
        ins.then_inc(E.sem, 1)
        E.count += 1
        self.ninst += 1
        c = E.count
        for b in reads:
            b.r[E.name] = c
        for b in writes:
            b.w = {E.name: c}
            b.r = {}
        return ins

    def dma(self, Q, reads, writes, out, in_, **kw):
        half = self.NDMA // 2
        if Q is self.pool:
            j = half + self.dnext_sw
            self.dnext_sw = (self.dnext_sw + 1) % half
        else:
            j = self.dnext
            self.dnext = (self.dnext + 1) % half
        key = ("d", j)
        if self.dval[j] and Q.seen.get(key, 0) < self.dval[j]:
            Q.eng.wait_ge(self.dsems[j], self.dval[j])
            Q.seen[key] = self.dval[j]
        self._need(Q, reads, writes)
        ins = Q.eng.dma_start(out=out, in_=in_, **kw)
        ins.then_inc(self.dsems[j], 16)
        self.dval[j] += 16
        v = self.dval[j]
        self.ninst += 1
        for b in reads:
            b.r[key] = v
        for b in writes:
            b.w = {key: v}
            b.r = {}
        return ins

    def barrier(self):
        for E in self.engs:
            for F in self.engs:
                if F is E or F.count == 0:
                    continue
                if E.seen.get(F.name, 0) < F.count:
                    E.eng.wait_ge(F.sem, F.count)
                    E.seen[F.name] = F.count
            for j in range(self.NDMA):
                key = ("d", j)
                if self.dval[j] and E.seen.get(key, 0) < self.dval[j]:
                    E.eng.wait_ge(self.dsems[j], self.dval[j])
                    E.seen[key] = self.dval[j]


class Rot:
    _uid = 0

    def __init__(self, nc, es, name, shape, dt, n=2):
        Rot._uid += 1
        self.t = [es.enter_context(nc.sbuf_tensor(f"{name}{i}_r{Rot._uid}", shape, dt)) for i in range(n)]
        self.b = [Buf() for _ in range(n)]
        self.i = -1
        self.n = n

    def next(self):
        self.i = (self.i + 1) % self.n
        return self.t[self.i], self.b[self.i]


_CONST_CACHE = {}


def host_consts():
    if _CONST_CACHE:
        return _CONST_CACHE
    c = {}
    f32 = np.float32
    c["ident32"] = np.eye(128, dtype=f32)
    c["identb"] = np.eye(128).astype(ml_dtypes.bfloat16)
    c["antib"] = np.eye(128)[::-1].copy().astype(ml_dtypes.bfloat16)
    c["onesb"] = np.full((128, 128), 1.0 / 128.0).astype(ml_dtypes.bfloat16)
    inv_r = (1.0 / (10000.0 ** np.linspace(0.0, 1.0, 64, dtype=f32))).astype(f32)
    tpos = np.arange(L, dtype=f32)
    ang = tpos[None, :] * inv_r[:, None]
    c["cosR"] = np.concatenate([np.cos(ang), np.cos(ang)], 0).astype(f32)
    c["sinR"] = np.concatenate([np.sin(ang), np.sin(ang)], 0).astype(f32)
    pr = np.zeros((128, 128), f32)
    for dp in range(64):
        pr[dp + 64, dp] = -1.0
        pr[dp, dp + 64] = 1.0
    c["permR"] = pr.astype(ml_dtypes.bfloat16)
    inv_a = (1.0 / (10000.0 ** (np.arange(16, dtype=f32) / 16))).astype(f32)
    rows = np.repeat(np.arange(L // 64, dtype=f32), 64)
    cols = np.tile(np.arange(64, dtype=f32), L // 64)
    angr = rows[None, :] * inv_a[:, None]
    angc = cols[None, :] * inv_a[:, None]
    c["cosA"] = np.concatenate([np.cos(angr), np.cos(angr), np.cos(angc), np.cos(angc)], 0).astype(f32)
    c["sinA"] = np.concatenate([np.sin(angr), np.sin(angr), np.sin(angc), np.sin(angc)], 0).astype(f32)
    pa = np.zeros((64, 64), f32)
    for base in (0, 32):
        for dp in range(16):
            pa[base + dp + 16, base + dp] = -1.0
            pa[base + dp, base + dp + 16] = 1.0
    c["permA"] = pa.astype(ml_dtypes.bfloat16)
    j = np.arange(128, dtype=f32)[:, None]
    i = np.arange(128, dtype=f32)[None, :]
    ks = 128.0 ** -0.5
    rt = np.zeros((128, 6, 128), f32)
    rt[:, 0] = np.maximum(i - j, 0.0)
    rt[:, 1] = (j <= i) * ks
    rt[:, 2] = np.maximum(j - i, 0.0)
    rt[:, 3] = (j > i) * ks
    rt[:, 4] = i + 1.0
    rt[:, 5] = 128.0 - i
    c["rtab"] = rt
    rc = np.zeros((128, 4), f32)
    rc[:, 0] = 127.0 - np.arange(128)
    rc[:, 1] = np.arange(128)
    rc[:, 2] = 128.0
    c["rcol"] = rc
    am = np.zeros((128, 2, 128), f32)
    am[:, 0] = (j >= i)
    am[:, 1] = (j <= i)
    c["amask"] = am.astype(ml_dtypes.bfloat16)
    def emb(Lq, width):
        t = np.linspace(0.0, 1.0, Lq, dtype=f32)
        bands = np.linspace(1e-4, 15, 16, dtype=f32)
        ang = (f32(2.0 * math.pi / Lq) * np.arange(Lq, dtype=f32)[:, None] * bands[None, :]).astype(f32)
        z = np.concatenate([t[:, None], np.cos(ang), -np.sin(ang)], -1).astype(f32)
        tidx = np.concatenate([np.arange(Lq - 1, -1, -1), np.arange(1, Lq)])
        e = np.zeros((33, width), f32)
        e[:, :2 * Lq - 1] = z[tidx].T
        tl = np.zeros((1, width), f32)
        tl[0, :2 * Lq - 1] = t[tidx]
        return e, tl
    c["embL"], c["tlinL"] = emb(L, 8192)
    c["embC"], c["tlinC"] = emb(LC, 512)
    deltas = np.abs(np.linspace(math.log(1e-2) / 1.5, math.log(1e-2) / 0.3, 512, dtype=f32)).astype(f32)
    c["ndelta"] = np.ascontiguousarray((-deltas).reshape(4, 128).T)
    _CONST_CACHE.update(c)
    return c


class Prog:
    def __init__(self, layers=(0, 1), phases=None, debug=False):
        self.layers = layers
        self.phases = phases
        self.debug = debug
        nc = self.nc = bass.Bass("TRN2", target_bir_lowering=False)
        self.fw = FW(nc)
        self.din = {}
        self.consts = host_consts()
        ges = self.ges = self.fw.es
        self.pb = [ges.enter_context(nc.psum_tensor(f"pb{i}", [128, 512], F32)) for i in range(8)]
        self.bpb = [Buf(excl=True) for _ in range(8)]
        okind = "ExternalOutput" if debug else "Internal"
        self.XS = nc.dram_tensor("XS", [NTOK, D], F32, kind=okind)
        self.bXS = [Buf() for _ in range(NT)]
        self.MIX = nc.dram_tensor("MIX", [3, 512, NTOK], BF16, kind=okind)
        self.bMIX = [[Buf() for _ in range(4)] for _ in range(3)]
        self.MODV = nc.dram_tensor("MODV", [2, 6 * D], F32, kind=okind)
        self.bMODV = Buf()
        self.GS = nc.dram_tensor("GS", [512, 8192], BF16, kind=okind)
        self.GC = nc.dram_tensor("GC", [512, 512], BF16, kind=okind)
        self.bGS = [Buf() for _ in range(4)]
        self.bGC = [Buf() for _ in range(4)]
        self.OUT = nc.dram_tensor("OUT", [L, D], F32, kind="ExternalOutput")
        self.bOUT = Buf()
        self.ident32, self.b_const = self.sbc(ges, "ident32", [128, 128], F32), Buf()
        self.identb = self.sbc(ges, "identb", [128, 128], BF16)
        self.antib = self.sbc(ges, "antib", [128, 128], BF16)
        self.onesb = self.sbc(ges, "onesb", [128, 128], BF16)
        for nm, t in (("ident32", self.ident32), ("identb", self.identb), ("antib", self.antib), ("onesb", self.onesb)):
            self.fw.dma(self.fw.sp, [], [self.b_const], t[:], self.cin(nm).ap())
        self.h1T = None

    def sbc(self, es, name, shape, dt):
        self._uid = getattr(self, "_uid", 0) + 1
        return es.enter_context(self.nc.sbuf_tensor(f"{name}_u{self._uid}", shape, dt))

    def inp(self, name, shape, dt=F32):
        if name not in self.din:
            self.din[name] = self.nc.dram_tensor(name, list(shape), dt, kind="ExternalInput")
        return self.din[name]

    def cin(self, name):
        a = self.consts[name]
        dt = BF16 if a.dtype == ml_dtypes.bfloat16 else F32
        return self.inp("k_" + name, a.shape, dt)

    def w(self, name):
        shapes = dict(
            w_mod=[DEPTH, D, 6 * D], b_mod=[DEPTH, 6 * D], norm1_g=[DEPTH, D], w_in=[DEPTH, D, IN_COLS],
            ret_decay_logit=[DEPTH, 8], attn_sink=[DEPTH, 8], hy_conv_w=[DEPTH, 3, 1536], hy_conv_b=[DEPTH, 1, 1536],
            hy_w1=[DEPTH, 33, 64], hy_b1=[DEPTH, 1, 64], hy_freq1=[DEPTH, 1, 64], hy_w2=[DEPTH, 64, 64],
            hy_b2=[DEPTH, 1, 64], hy_freq2=[DEPTH, 1, 64], hy_w3=[DEPTH, 64, 1024], hy_skip=[DEPTH, 1, 512],
            w_branch=[DEPTH, 1536, D], b_gate=[DEPTH, 1, 3072], w_out=[DEPTH, D, D], norm2_g=[DEPTH, D],
            router_w=[DEPTH, D, NEXP], router_b=[DEPTH, NEXP], moe_w1=[DEPTH, NEXP, D, 2 * DFF],
            moe_b1=[DEPTH, NEXP, 2 * DFF], moe_w2=[DEPTH, NEXP, DFF, D], moe_b2=[DEPTH, NEXP, D],
            final_norm_g=[1, D], x=[L, D], ctx=[LC, D], cc=[16, 128])
        return self.inp(name, shapes[name])

    def bc_ap(self, t, off, n, parts=128):
        return bass.AP(tensor=t, offset=off, ap=[[0, parts], [1, n]])

    def V(self, r, w, f):
        return self.fw.op(self.fw.dve, r, w, f)

    def A(self, r, w, f):
        return self.fw.op(self.fw.act, r, w, f)

    def G(self, r, w, f):
        return self.fw.op(self.fw.pool, r, w, f)

    def T(self, r, w, f):
        return self.fw.op(self.fw.pe, r, w, f)

    def ld(self, r, w, out, in_, q=None):
        return self.fw.dma(q or self.fw.sp, r, w, out, in_)

    def ldcast(self, r, w, out, in_):
        return self.fw.dma(self.fw.pool, r, w, out, in_)

    def wslice(self, name, l, c0, n):
        t = self.w(name)
        return t.ap()[l, :, c0:c0 + n].rearrange("(k p) n -> p k n", p=128)

    def load_cols(self, es, name, src_rows_ap, R, N, bank=7, rows_es=None):
        nc = self.nc
        n = min(N, 128)
        nch = (N + 127) // 128
        dst = self.sbc(es, name, [128, nch, R], F32)
        bdst = Buf()
        rows = self.sbc(rows_es or es, name + "_r", [R, N], F32)
        brow = Buf()
        self.ld([], [brow], rows[:], src_rows_ap)
        for j in range(nch):
            self.T([brow, self.b_const], [self.bpb[bank]], lambda: nc.tensor.transpose(
                out=self.pb[bank][0:n, j * R:(j + 1) * R], in_=rows[0:R, j * 128:j * 128 + n], identity=self.ident32[0:R, 0:R]))
        self.V([self.bpb[bank]], [bdst], lambda: nc.vector.tensor_copy(
            out=dst[0:n, :, :], in_=self.pb[bank][0:n, 0:nch * R].rearrange("p (c r) -> p c r", r=R)))
        return dst, bdst

    def proj(self, bank, Wt, bW, c0, m, tok0, n, out_p0=0):
        nc = self.nc
        for k in range(8):
            self.T([bW, self.bh1T], [self.bpb[bank]], lambda: nc.tensor.matmul(
                self.pb[bank][out_p0:out_p0 + m, 0:n], lhsT=Wt[:, k, c0:c0 + m], rhs=self.h1T[:, k, tok0:tok0 + n],
                start=(k == 0), stop=(k == 7)))

    def run_phase(self, name):
        return self.phases is None or name in self.phases

    def build(self):
        nc, fw = self.nc, self.fw
        try:
            self._build_body()
        except Truncate:
            print("TRUNCATED at", fw.ninst)
        fw._need(fw.sp, [self.bOUT] + self.bXS, [])
        fw.barrier()
        return nc

    def _build_body(self):
        nc, fw = self.nc, self.fw
        if self.run_phase("init"):
            for t0 in range(0, 32, 8):
                self.ld([], self.bXS[t0:t0 + 8], self.XS.ap()[t0 * 128:(t0 + 8) * 128, :], self.w("x").ap()[t0 * 128:(t0 + 8) * 128, :])
            self.ld([], self.bXS[32:34], self.XS.ap()[L:L + LC, :], self.w("ctx").ap())
        for l in self.layers:
            last = (l == DEPTH - 1)
            if self.run_phase("mod"):
                self.phase_mod(l)
                fw.barrier()
            with ExitStack() as mes:
                if any(self.run_phase(p) for p in ("h1", "ret", "att", "hy", "merge")):
                    self.h1T = self.sbc(mes, "h1T", [128, 8, NTOK], BF16)
                    self.bh1T = Buf()
                    self.phase_norm_T(l)
                    fw.barrier()
                    if self.debug:
                        dbg = nc.dram_tensor(f"DBG_h1T{l}", [128, 8, NTOK], BF16, kind="ExternalOutput")
                        self.ld([self.bh1T], [Buf()], dbg.ap(), self.h1T[:])
                        fw.barrier()
                if self.run_phase("ret"):
                    self.phase_ret(l, last)
                    fw.barrier()
                if self.run_phase("att"):
                    self.phase_att(l, last)
                    fw.barrier()
                if self.run_phase("hy"):
                    self.phase_hy(l, last)
                    fw.barrier()
                if self.run_phase("merge"):
                    self.phase_merge(l, last)
                    fw.barrier()
            if self.run_phase("moe"):
                self.phase_moe(l, last)
                fw.barrier()

    def phase_mod(self, l):
        nc = self.nc
        with ExitStack() as es:
            crow = self.sbc(es, "crow", [16, 128], F32); bcrow = Buf()
            self.ld([], [bcrow], crow[:], self.w("cc").ap())
            cs = self.sbc(es, "cs", [128, 16], F32); bcs = Buf()
            self.T([bcrow, self.b_const], [self.bpb[0]], lambda: nc.tensor.transpose(
                out=self.pb[0][:, 0:16], in_=crow[:, :], identity=self.ident32[0:16, 0:16]))
            self.A([self.bpb[0]], [bcs], lambda: nc.scalar.activation(out=cs[:], in_=self.pb[0][:, 0:16], func=AF.Silu))
            bm = self.sbc(es, "bm", [2, 6 * D], F32); bbm = Buf()
            for r in range(2):
                self.ld([], [bbm], bm[r:r + 1, :], self.w("b_mod").ap()[l:l + 1, :])
            modv = self.sbc(es, "modv", [2, 6 * D], F32); bmodv = Buf()
            wr = Rot(nc, es, "wmod", [128, 8, 512], F32, 2)
            for cg in range(12):
                wt, bw = wr.next()
                self.ld([], [bw], wt[:], self.w("w_mod").ap()[l, :, cg * 512:(cg + 1) * 512].rearrange("(k p) n -> p k n", p=128))
                bank = cg % 2
                for k in range(8):
                    self.T([bw, bcs], [self.bpb[bank]], lambda: nc.tensor.matmul(
                        self.pb[bank][0:2, :], lhsT=cs[:, k:16:8], rhs=wt[:, k, :], start=(k == 0), stop=(k == 7)))
                self.V([self.bpb[bank], bbm], [bmodv], lambda: nc.vector.tensor_tensor(
                    out=modv[:, cg * 512:(cg + 1) * 512], in0=self.pb[bank][0:2, :], in1=bm[:, cg * 512:(cg + 1) * 512], op=ALU.add))
            self.ld([bmodv], [self.bMODV], self.MODV.ap(), modv[:])

    def mod_tables(self, es, l, gname, off_sh, off_sc, pfx, tmp_es=None):
        nc = self.nc
        gb = self.sbc(tmp_es or es, pfx + "gb", [128, D], F32); bg = Buf()
        self.ld([], [bg], gb[:], self.bc_ap(self.w(gname), l * D, D))
        Al, Sl = [], []
        bt = Buf()
        for r in range(2):
            a = self.sbc(es, f"{pfx}A{r}", [128, D], F32)
            s = self.sbc(es, f"{pfx}S{r}", [128, D], F32)
            self.ld([self.bMODV], [bt], a[:], self.bc_ap(self.MODV, r * 6 * D + off_sc, D))
            self.ld([self.bMODV], [bt], s[:], self.bc_ap(self.MODV, r * 6 * D + off_sh, D))
            self.V([bt, bg], [bt], lambda: nc.vector.scalar_tensor_tensor(
                out=a[:], in0=a[:], scalar=1.0, in1=gb[:], op0=ALU.add, op1=ALU.mult))
            Al.append(a); Sl.append(s)
        return Al, Sl, bt

    def norm_tile(self, xt, bx, xn, bxn, A, S, bt, scr, add_on_dve=False):
        nc = self.nc
        junk, bj, st, bst = scr
        self.V([], [bst], lambda: nc.vector.memset(st[:, 0:1], 0.0))
        self.A([bx], [bj, bst], lambda: nc.scalar.activation(out=junk[:], in_=xt, func=AF.Square, accum_out=st[:, 0:1]))
        self.V([bst], [bst], lambda: nc.vector.tensor_scalar(out=st[:, 1:2], in0=st[:, 0:1], scalar1=1.0 / D, scalar2=EPS, op0=ALU.mult, op1=ALU.add))
        self.A([bst], [bst], lambda: nc.scalar.activation(out=st[:, 2:3], in_=st[:, 1:2], func=AF.Sqrt))
        self.V([bst], [bst], lambda: nc.vector.reciprocal(out=st[:, 3:4], in_=st[:, 2:3]))
        self.V([bx, bst, bt], [bxn], lambda: nc.vector.scalar_tensor_tensor(
            out=xn, in0=xt, scalar=st[:, 3:4], in1=A[:], op0=ALU.mult, op1=ALU.mult))
        if add_on_dve:
            self.V([bxn, bt], [bxn], lambda: nc.vector.tensor_tensor(out=xn, in0=xn, in1=S[:], op=ALU.add))
        else:
            self.G([bxn, bt], [bxn], lambda: nc.gpsimd.tensor_tensor(out=xn, in0=xn, in1=S[:], op=ALU.add))

    def phase_norm_T(self, l):
        nc = self.nc
        with ExitStack() as es:
            Al, Sl, bt = self.mod_tables(es, l, "norm1_g", 0, D, "n1")
            xr = Rot(nc, es, "xt", [128, D], F32, 2)
            xnr = Rot(nc, es, "xn", [128, D], F32, 2)
            junk = self.sbc(es, "junk", [128, D], F32); bj = Buf()
            str_ = Rot(nc, es, "st", [128, 4], F32, 2)
            for t in range(NT):
                r = 0 if t < 32 else 1
                xt, bx = xr.next(); xn, bxn = xnr.next(); st, bst = str_.next()
                self.ld([self.bXS[t]], [bx], xt[:], self.XS.ap()[t * 128:(t + 1) * 128, :])
                self.norm_tile(xt[:], bx, xn[:], bxn, Al[r], Sl[r], bt, (junk, bj, st, bst))
                for half in range(2):
                    bank = (2 * t + half) % 4
                    for kk in range(4):
                        k = half * 4 + kk
                        self.T([bxn, self.b_const], [self.bpb[bank]], lambda: nc.tensor.transpose(
                            out=self.pb[bank][:, kk * 128:(kk + 1) * 128], in_=xn[:, k * 128:(k + 1) * 128], identity=self.ident32[:]))
                    cp = self.A if half == 0 else self.V
                    eng = nc.scalar.copy if half == 0 else nc.vector.tensor_copy
                    cp([self.bpb[bank]], [self.bh1T], lambda: eng(
                        out=self.h1T[:, half * 4:half * 4 + 4, t * 128:(t + 1) * 128],
                        in_=self.pb[bank][:, :].rearrange("p (a b) -> p a b", a=4)))

    def rope_fm(self, bank_a, bank_b, m, n, tok0, cosT, sinT, perm, btab, rs, out_ap, bout):
        nc = self.nc
        (asb_r, t1_r, t2_r) = rs
        asb, basb = asb_r.next(); t1, bt1 = t1_r.next(); t2, bt2 = t2_r.next()
        self.A([self.bpb[bank_a]], [basb], lambda: nc.scalar.copy(out=asb[0:m, 0:n], in_=self.pb[bank_a][0:m, 0:n]))
        self.T([basb, btab], [self.bpb[bank_b]], lambda: nc.tensor.matmul(
            self.pb[bank_b][0:m, 0:n], lhsT=perm[0:m, 0:m], rhs=asb[0:m, 0:n], start=True, stop=True))
        self.V([self.bpb[bank_a], btab], [bt1], lambda: nc.vector.tensor_tensor(
            out=t1[0:m, 0:n], in0=self.pb[bank_a][0:m, 0:n], in1=cosT[0:m, tok0:tok0 + n], op=ALU.mult))
        self.V([self.bpb[bank_b], btab], [bt2], lambda: nc.vector.tensor_tensor(
            out=t2[0:m, 0:n], in0=self.pb[bank_b][0:m, 0:n], in1=sinT[0:m, tok0:tok0 + n], op=ALU.mult))
        self.G([bt1, bt2], [bout], lambda: nc.gpsimd.tensor_tensor(out=out_ap, in0=t1[0:m, 0:n], in1=t2[0:m, 0:n], op=ALU.add))

    def tok_groups(self, with_ctx=True):
        g = [(i * 512, 512) for i in range(8)]
        if with_ctx:
            g.append((L, LC))
        return g

    def phase_ret(self, l, last):
        nc = self.nc
        with ExitStack() as es:
            cosT = self.sbc(es, "cosR", [128, L], F32); sinT = self.sbc(es, "sinR", [128, L], F32)
            perm = self.sbc(es, "permR", [128, 128], BF16)
            rtab = self.sbc(es, "rtab", [128, 6, 128], F32); rcol = self.sbc(es, "rcol", [128, 4], F32)
            btab = Buf()
            for t, nm in ((cosT, "cosR"), (sinT, "sinR"), (perm, "permR"), (rtab, "rtab"), (rcol, "rcol")):
                self.ld([], [btab], t[:], self.cin(nm).ap())
            lg = self.sbc(es, "lg", [128, 8], F32); blg = Buf()
            self.ld([], [blg], lg[:], self.bc_ap(self.w("ret_decay_logit"), l * 8, 8))
            self.A([blg], [blg], lambda: nc.scalar.activation(out=lg[:], in_=lg[:], func=AF.Exp, scale=-1.0))
            self.A([blg], [blg], lambda: nc.scalar.activation(out=lg[:], in_=lg[:], func=AF.Ln, bias=1.0))
            self.V([blg], [blg], lambda: nc.vector.tensor_scalar(out=lg[:], in0=lg[:], scalar1=-1.0, scalar2=None, op0=ALU.mult))
            qT = self.sbc(es, "qT", [128, NTOK], BF16); bq = Buf()
            kT = self.sbc(es, "kT", [128, NTOK], BF16); bk = Buf()
            sg = self.sbc(es, "sg", [128, NTOK], BF16); bsg = Buf()
            vtm = self.sbc(es, "vtm", [128, NT, 128], BF16); bv = Buf()
            kwf = self.sbc(es, "kwf", [128, NT, 128], BF16); bkwf = Buf()
            kwb = self.sbc(es, "kwb", [128, NT, 128], BF16); bkwb = Buf()
            sbp = self.sbc(es, "sbp", [128, NT, 128], BF16); bsbp = Buf()
            S32 = self.sbc(es, "S32", [128, 128], F32); bS = Buf()
            DT = self.sbc(es, "DT", [128, 128], F32); bDT = Buf()
            dtmp = self.sbc(es, "dtmp", [128, 128], F32)
            wrd = self.sbc(es, "wrd", [128, 2, 128], BF16); bwrd = Buf()
            wcol = self.sbc(es, "wcol", [128, 4], F32); bwcol = Buf()
            W4 = self.sbc(es, "W4", [128, 4, 8, 128], BF16); bW = Buf()
            rs = (Rot(nc, es, "asb", [128, 512], BF16, 2), Rot(nc, es, "t1", [128, 512], F32, 2), Rot(nc, es, "t2", [128, 512], F32, 2))
            sfr = Rot(nc, es, "sfb", [128, 128], BF16, 6)
            psb_r = Rot(nc, es, "psb", [128, 512], BF16, 2)
            qw_r = Rot(nc, es, "qw", [128, 2, 512], BF16, 2)
            ysq_r = Rot(nc, es, "ysq", [128, 512], BF16, 2)
            rst_r = Rot(nc, es, "rst", [128, 512], F32, 2)
            yt_r = Rot(nc, es, "yt", [128, 512], F32, 2)
            ro_r = Rot(nc, es, "ro", [128, 512], BF16, 2)
            ks = 128.0 ** -0.5
            for h in range(4):
                lgf = lg[:, h:h + 1]; lgb = lg[:, 4 + h:5 + h]
                self.A([blg, btab], [bDT], lambda: nc.scalar.activation(out=DT[:], in_=rtab[:, 0, :], func=AF.Exp, scale=lgf))
                self.V([bDT, btab], [bDT], lambda: nc.vector.tensor_tensor(out=DT[:], in0=DT[:], in1=rtab[:, 1, :], op=ALU.mult))
                self.A([blg, btab], [bDT], lambda: nc.scalar.activation(out=dtmp[:], in_=rtab[:, 2, :], func=AF.Exp, scale=lgb))
                self.V([bDT, btab], [bDT], lambda: nc.vector.tensor_tensor(out=dtmp[:], in0=dtmp[:], in1=rtab[:, 3, :], op=ALU.mult))
                self.V([bDT], [bDT], lambda: nc.vector.tensor_tensor(out=DT[:], in0=DT[:], in1=dtmp[:], op=ALU.add))
                self.A([blg, btab], [bwrd], lambda: nc.scalar.activation(out=wrd[:, 0, :], in_=rtab[:, 4, :], func=AF.Exp, scale=lgf))
                self.A([blg, btab], [bwrd], lambda: nc.scalar.activation(out=wrd[:, 1, :], in_=rtab[:, 5, :], func=AF.Exp, scale=lgb))
                self.A([blg, btab], [bwcol], lambda: nc.scalar.activation(out=wcol[:, 0:1], in_=rcol[:, 0:1], func=AF.Exp, scale=lgf))
                self.A([blg, btab], [bwcol], lambda: nc.scalar.activation(out=wcol[:, 1:2], in_=rcol[:, 1:2], func=AF.Exp, scale=lgb))
                self.A([blg, btab], [bwcol], lambda: nc.scalar.activation(out=wcol[:, 2:3], in_=rcol[:, 2:3], func=AF.Exp, scale=lgf))
                self.A([blg, btab], [bwcol], lambda: nc.scalar.activation(out=wcol[:, 3:4], in_=rcol[:, 2:3], func=AF.Exp, scale=lgb))
                self.V([bwcol], [bwcol], lambda: nc.vector.tensor_scalar(out=wcol[:, 0:2], in0=wcol[:, 0:2], scalar1=ks, scalar2=None, op0=ALU.mult))
                for i, c0 in enumerate((C_RQ, C_RK, C_RV, C_RG)):
                    self.ldcast([], [bW], W4[:, i, :, :], self.wslice("w_in", l, c0 + h * 128, 128))
                for (tok0, n) in self.tok_groups():
                    lat = tok0 < L
                    for i, (dst, bdst) in enumerate(((qT, bq), (kT, bk))):
                        bank = i
                        self.proj(bank, W4[:, i], bW, 0, 128, tok0, n)
                        if lat:
                            self.rope_fm(bank, 2 + i, 128, n, tok0, cosT, sinT, perm, btab, rs, dst[:, tok0:tok0 + n], bdst)
                        else:
                            self.A([self.bpb[bank]], [bdst], lambda: nc.scalar.copy(out=dst[:, tok0:tok0 + n], in_=self.pb[bank][:, 0:n]))
                    self.proj(4, W4[:, 3], bW, 0, 128, tok0, n)
                    self.A([self.bpb[4]], [bsg], lambda: nc.scalar.activation(out=sg[:, tok0:tok0 + n], in_=self.pb[4][:, 0:n], func=AF.Silu))
                    nt = n // 128
                    for j in range(nt):
                        for k in range(8):
                            self.T([bW, self.bh1T], [self.bpb[5]], lambda: nc.tensor.matmul(
                                self.pb[5][:, j * 128:(j + 1) * 128], lhsT=self.h1T[:, k, tok0 + j * 128:tok0 + (j + 1) * 128],
                                rhs=W4[:, 2, k, :], start=(k == 0), stop=(k == 7)))
                    self.V([self.bpb[5]], [bv], lambda: nc.vector.tensor_copy(
                        out=vtm[:, tok0 // 128:tok0 // 128 + nt, :], in_=self.pb[5][:, 0:n].rearrange("p (a b) -> p a b", b=128)))
                for t0 in range(0, NT, 4):
                    nt = min(4, NT - t0)
                    bank = 6 + (t0 // 4) % 2
                    pbb = self.pb[bank][:, :].bitcast(BF16)
                    for j in range(nt):
                        self.T([bk, self.b_const], [self.bpb[bank]], lambda: nc.tensor.transpose(
                            out=pbb[:, j * 128:(j + 1) * 128], in_=kT[:, (t0 + j) * 128:(t0 + j + 1) * 128], identity=self.identb[:]))
                    src = pbb[:, 0:nt * 128].rearrange("p (a b) -> p a b", b=128)
                    self.V([self.bpb[bank], bwcol], [bkwf], lambda: nc.vector.tensor_scalar(
                        out=kwf[:, t0:t0 + nt, :], in0=src, scalar1=wcol[:, 0:1], scalar2=None, op0=ALU.mult))
                    self.A([self.bpb[bank], bwcol], [bkwb], lambda: nc.scalar.activation(
                        out=kwb[:, t0:t0 + nt, :], in_=src, func=AF.Identity, scale=wcol[:, 1:2]))
                self.V([], [bS], lambda: nc.vector.memset(S32[:], 0.0))
                for idx, n_ in enumerate([33, 32] + list(range(31, -1, -1))):
                    bank = 6 + idx % 2
                    self.V([bS], [bsbp], lambda: nc.vector.tensor_copy(out=sbp[:, n_, :], in_=S32[:]))
                    self.T([bkwb, bv], [self.bpb[bank]], lambda: nc.tensor.matmul(
                        self.pb[bank][:, 0:128], lhsT=kwb[:, n_, :], rhs=vtm[:, n_, :], start=True, stop=True))
                    self.V([self.bpb[bank], bS, bwcol], [bS], lambda: nc.vector.scalar_tensor_tensor(
                        out=S32[:], in0=S32[:], scalar=wcol[:, 3:4], in1=self.pb[bank][:, 0:128], op0=ALU.mult, op1=ALU.add))
                self.V([], [bS], lambda: nc.vector.memset(S32[:], 0.0))
                groups = [(32, 2)] + [(c0, 4) for c0 in range(0, 32, 4)]
                for gi, (c0, ncnk) in enumerate(groups):
                    isctx = c0 >= 32
                    want_out = (not isctx) or (not last)
                    tok0 = c0 * 128; n = ncnk * 128
                    sfs = []
                    for c in range(ncnk):
                        n_ = c0 + c
                        sf, bsf = sfr.next(); sfs.append((sf, bsf))
                        self.V([bS], [bsf], lambda: nc.vector.tensor_copy(out=sf[:], in_=S32[:]))
                        bank = 6 + c % 2
                        self.T([bkwf, bv], [self.bpb[bank]], lambda: nc.tensor.matmul(
                            self.pb[bank][:, 0:128], lhsT=kwf[:, n_, :], rhs=vtm[:, n_, :], start=True, stop=True))
                        self.V([self.bpb[bank], bS, bwcol], [bS], lambda: nc.vector.scalar_tensor_tensor(
                            out=S32[:], in0=S32[:], scalar=wcol[:, 2:3], in1=self.pb[bank][:, 0:128], op0=ALU.mult, op1=ALU.add))
                    if not want_out:
                        continue
                    bs_, bo_ = (0, 1) if gi % 2 == 0 else (2, 3)
                    for c in range(ncnk):
                        self.T([bk, bq], [self.bpb[bs_]], lambda: nc.tensor.matmul(
                            self.pb[bs_][:, c * 128:(c + 1) * 128], lhsT=kT[:, tok0 + c * 128:tok0 + (c + 1) * 128],
                            rhs=qT[:, tok0 + c * 128:tok0 + (c + 1) * 128], start=True, stop=True))
                    psb, bpsb = psb_r.next()
                    self.V([self.bpb[bs_], bDT], [bpsb], lambda: nc.vector.tensor_tensor(
                        out=psb[:, 0:n].rearrange("p (a b) -> p a b", b=128), in0=self.pb[bs_][:, 0:n].rearrange("p (a b) -> p a b", b=128),
                        in1=bass.AP(tensor=DT[:].tensor, offset=DT[:].offset, ap=[list(DT[:].ap[0]), [0, ncnk], [1, 128]]), op=ALU.mult))
                    qw, bqw = qw_r.next()
                    for d_ in range(2):
                        wr_ap = wrd[:, d_, :]
                        self.G([bq, bwrd], [bqw], lambda: nc.gpsimd.tensor_tensor(
                            out=qw[:, d_, 0:n].rearrange("p (a b) -> p a b", b=128), in0=qT[:, tok0:tok0 + n].rearrange("p (a b) -> p a b", b=128),
                            in1=bass.AP(tensor=wr_ap.tensor, offset=wr_ap.offset, ap=[list(wr_ap.ap[0]), [0, ncnk], [1, 128]]), op=ALU.mult))
                    for c in range(ncnk):
                        n_ = c0 + c
                        o = self.pb[bo_][:, c * 128:(c + 1) * 128]
                        self.T([bv, bpsb], [self.bpb[bo_]], lambda: nc.tensor.matmul(o, lhsT=vtm[:, n_, :], rhs=psb[:, c * 128:(c + 1) * 128], start=True, stop=False))
                        self.T([sfs[c][1], bqw], [self.bpb[bo_]], lambda: nc.tensor.matmul(o, lhsT=sfs[c][0][:], rhs=qw[:, 0, c * 128:(c + 1) * 128], start=False, stop=False))
                        self.T([bsbp, bqw], [self.bpb[bo_]], lambda: nc.tensor.matmul(o, lhsT=sbp[:, n_, :], rhs=qw[:, 1, c * 128:(c + 1) * 128], start=False, stop=True))
                    ysq, bysq = ysq_r.next()
                    self.A([self.bpb[bo_]], [bysq], lambda: nc.scalar.activation(out=ysq[:, 0:n], in_=self.pb[bo_][:, 0:n], func=AF.Square))
                    bm_ = 4 + gi % 2
                    self.T([bysq, self.b_const], [self.bpb[bm_]], lambda: nc.tensor.matmul(
                        self.pb[bm_][:, 0:n], lhsT=self.onesb[:], rhs=ysq[:, 0:n], start=True, stop=True))
                    rst, brst = rst_r.next()
                    self.V([self.bpb[bm_]], [brst], lambda: nc.vector.tensor_scalar(out=rst[:, 0:n], in0=self.pb[bm_][:, 0:n], scalar1=EPS, scalar2=None, op0=ALU.add))
                    self.A([brst], [brst], lambda: nc.scalar.activation(out=rst[:, 0:n], in_=rst[:, 0:n], func=AF.Sqrt))
                    self.V([brst], [brst], lambda: nc.vector.reciprocal(out=rst[:, 0:n], in_=rst[:, 0:n]))
                    yt, byt = yt_r.next()
                    self.V([self.bpb[bo_], brst], [byt], lambda: nc.vector.tensor_tensor(out=yt[:, 0:n], in0=self.pb[bo_][:, 0:n], in1=rst[:, 0:n], op=ALU.mult))
                    ro, bro = ro_r.next()
                    self.G([byt, bsg], [bro], lambda: nc.gpsimd.tensor_tensor(out=ro[:, 0:n], in0=yt[:, 0:n], in1=sg[:, tok0:tok0 + n], op=ALU.mult))
                    self.ld([bro], [self.bMIX[0][h]], self.MIX.ap()[0, h * 128:(h + 1) * 128, tok0:tok0 + n], ro[:, 0:n])

    def phase_att(self, l, last):
        nc = self.nc
        with ExitStack() as es:
            cosT = self.sbc(es, "cosA", [64, L], F32); sinT = self.sbc(es, "sinA", [64, L], F32)
            perm = self.sbc(es, "permA", [64, 64], BF16)
            amask = self.sbc(es, "amask", [128, 2, 128], BF16)
            btab = Buf()
            for t, nm in ((cosT, "cosA"), (sinT, "sinA"), (perm, "permA"), (amask, "amask")):
                self.ld([], [btab], t[:], self.cin(nm).ap())
            esink = self.sbc(es, "esink", [128, 8], F32); bes = Buf()
            self.ld([], [bes], esink[:], self.bc_ap(self.w("attn_sink"), l * 8, 8))
            self.A([bes], [bes], lambda: nc.scalar.activation(out=esink[:], in_=esink[:], func=AF.Exp))
            Wq = self.sbc(es, "Wq", [128, 8, 256], BF16); Wk = self.sbc(es, "Wk", [128, 8, 64], BF16); Wv = self.sbc(es, "Wv", [128, 8, 64], BF16)
            bW = Buf()
            kT = self.sbc(es, "akT", [64, NTOK], BF16); bk = Buf()
            qT = self.sbc(es, "aqT", [64, NT, 4, 128], BF16); bq = Buf()
            v1 = self.sbc(es, "av1", [128, NT, 65], BF16); bv = Buf()
            rs = (Rot(nc, es, "asb", [64, 512], BF16, 2), Rot(nc, es, "t1", [64, 512], F32, 2), Rot(nc, es, "t2", [64, 512], F32, 2))
            e_r = Rot(nc, es, "E", [128, 512], BF16, 10)
            den_r = Rot(nc, es, "den", [128, 8], F32, 2)
            atm_r = Rot(nc, es, "atm", [128, 256], F32, 2)
            ao_r = Rot(nc, es, "ao", [128, 2, 512], BF16, 2)
            self.V([], [bv], lambda: nc.vector.memset(v1[:], 1.0))
            for g in range(2):
                self.ldcast([], [bW], Wq[:], self.wslice("w_in", l, C_AQ + g * 256, 256))
                self.ldcast([], [bW], Wk[:], self.wslice("w_in", l, C_AK + g * 64, 64))
                self.ldcast([], [bW], Wv[:], self.wslice("w_in", l, C_AV + g * 64, 64))
                for (tok0, n) in self.tok_groups():
                    lat = tok0 < L
                    nt = n // 128
                    self.proj(0, Wk, bW, 0, 64, tok0, n)
                    if lat:
                        self.rope_fm(0, 2, 64, n, tok0, cosT, sinT, perm, btab, rs, kT[:, tok0:tok0 + n], bk)
                    else:
                        self.A([self.bpb[0]], [bk], lambda: nc.scalar.copy(out=kT[:, tok0:tok0 + n], in_=self.pb[0][0:64, 0:n]))
                    for hh in range(4):
                        bank = hh % 2
                        self.proj(bank, Wq, bW, hh * 64, 64, tok0, n)
                        dst = qT[:, tok0 // 128:tok0 // 128 + nt, hh, :]
                        if lat:
                            self._rope_q(bank, 2 + hh % 2, n, tok0, cosT, sinT, perm, btab, rs, dst, bq)
                        else:
                            self.A([self.bpb[bank]], [bq], lambda: nc.scalar.copy(out=dst, in_=self.pb[bank][0:64, 0:n].rearrange("p (a b) -> p a b", b=128)))
                    for j in range(nt):
                        for k in range(8):
                            self.T([bW, self.bh1T], [self.bpb[4]], lambda: nc.tensor.matmul(
                                self.pb[4][:, j * 64:(j + 1) * 64], lhsT=self.h1T[:, k, tok0 + j * 128:tok0 + (j + 1) * 128],
                                rhs=Wv[:, k, :], start=(k == 0), stop=(k == 7)))
                    self.V([self.bpb[4]], [bv], lambda: nc.vector.tensor_copy(
                        out=v1[:, tok0 // 128:tok0 // 128 + nt, 0:64], in_=self.pb[4][:, 0:nt * 64].rearrange("p (a b) -> p a b", b=64)))
                qtiles = list(range(32)) + ([] if last else [32, 33])
                for qi, qt in enumerate(qtiles):
                    if qt < 32:
                        keys = ([(qt - 1, 0)] if qt > 0 else []) + [(qt, None)] + ([(qt + 1, 1)] if qt < 31 else []) + [(32, None), (33, None)]
                    else:
                        keys = [(32, None), (33, None)]
                    bo_ = 5 + qi % 2
                    Es = []
                    for idx, (kt, mk) in enumerate(keys):
                        bs_ = idx
                        self.T([bk, bq], [self.bpb[bs_]], lambda: nc.tensor.matmul(
                            self.pb[bs_][:, :], lhsT=kT[:, kt * 128:(kt + 1) * 128], rhs=qT[:, qt, :, :].rearrange("p a b -> p (a b)"), start=True, stop=True))
                        E, bE = e_r.next()
                        self.A([self.bpb[bs_]], [bE], lambda: nc.scalar.activation(out=E[:], in_=self.pb[bs_][:, :], func=AF.Exp, scale=0.125))
                        if mk is not None:
                            m_ap = amask[:, mk, :]
                            self.G([bE, btab], [bE], lambda: nc.gpsimd.tensor_tensor(
                                out=E[:].rearrange("p (a b) -> p a b", b=128), in0=E[:].rearrange("p (a b) -> p a b", b=128),
                                in1=bass.AP(tensor=m_ap.tensor, offset=m_ap.offset, ap=[list(m_ap.ap[0]), [0, 4], [1, 128]]), op=ALU.mult))
                        Es.append((E, bE))
                    for hh in range(4):
                        for idx, (kt, mk) in enumerate(keys):
                            E, bE = Es[idx]
                            self.T([bE, bv], [self.bpb[bo_]], lambda: nc.tensor.matmul(
                                self.pb[bo_][:, hh * 65:(hh + 1) * 65], lhsT=E[:, hh * 128:(hh + 1) * 128], rhs=v1[:, kt, :],
                                start=(idx == 0), stop=(idx == len(keys) - 1)))
                    den, bden = den_r.next()
                    o3 = self.pb[bo_][:, 0:260].rearrange("p (a b) -> p a b", b=65)
                    self.V([self.bpb[bo_], bes], [bden], lambda: nc.vector.tensor_tensor(
                        out=den[:, 0:4], in0=self.pb[bo_][:, 64:260:65], in1=esink[:, g * 4:(g + 1) * 4], op=ALU.add))
                    self.V([bden], [bden], lambda: nc.vector.reciprocal(out=den[:, 4:8], in_=den[:, 0:4]))
                    atm, batm = atm_r.next()
                    self.V([self.bpb[bo_], bden], [batm], lambda: nc.vector.tensor_tensor(
                        out=atm[:].rearrange("p (a b) -> p a b", b=64), in0=o3[:, :, 0:64], in1=bass.AP(tensor=den[:].tensor, offset=den[:, 4:8].offset, ap=[list(den[:].ap[0]), [1, 4], [0, 64]]), op=ALU.mult))
                    j = qi % 4
                    if j == 0:
                        ao, bao = ao_r.next()
                        qt0 = qt
                    for c in range(2):
                        self.T([batm, self.b_const], [self.bpb[7]], lambda: nc.tensor.transpose(
                            out=self.pb[7][:, c * 256 + (j % 2) * 128:c * 256 + (j % 2) * 128 + 128], in_=atm[:, c * 128:(c + 1) * 128], identity=self.ident32[:]))
                    if j % 2 == 1 or qi == len(qtiles) - 1:
                        nn = (j % 2 + 1) * 128
                        off = (j // 2) * 256
                        for c in range(2):
                            cp = self.A if c == 0 else self.V
                            eng = nc.scalar.copy if c == 0 else nc.vector.tensor_copy
                            cp([self.bpb[7]], [bao], lambda: eng(out=ao[:, c, off:off + nn], in_=self.pb[7][:, c * 256:c * 256 + nn]))
                    if j == 3 or qi == len(qtiles) - 1:
                        nn = (j + 1) * 128
                        for c in range(2):
                            self.ld([bao], [self.bMIX[1][2 * g + c]], self.MIX.ap()[1, (2 * g + c) * 128:(2 * g + c + 1) * 128, qt0 * 128:qt0 * 128 + nn], ao[:, c, 0:nn])

    def _rope_q(self, bank_a, bank_b, n, tok0, cosT, sinT, perm, btab, rs, dst, bq):
        nc = self.nc
        asb, basb = rs[0].next(); t1, bt1 = rs[1].next(); t2, bt2 = rs[2].next()
        self.A([self.bpb[bank_a]], [basb], lambda: nc.scalar.copy(out=asb[0:64, 0:n], in_=self.pb[bank_a][0:64, 0:n]))
        self.T([basb, btab], [self.bpb[bank_b]], lambda: nc.tensor.matmul(
            self.pb[bank_b][0:64, 0:n], lhsT=perm[0:64, 0:64], rhs=asb[0:64, 0:n], start=True, stop=True))
        self.V([self.bpb[bank_a], btab], [bt1], lambda: nc.vector.tensor_tensor(
            out=t1[0:64, 0:n], in0=self.pb[bank_a][0:64, 0:n], in1=cosT[0:64, tok0:tok0 + n], op=ALU.mult))
        self.V([self.bpb[bank_b], btab], [bt2], lambda: nc.vector.tensor_tensor(
            out=t2[0:64, 0:n], in0=self.pb[bank_b][0:64, 0:n], in1=sinT[0:64, tok0:tok0 + n], op=ALU.mult))
        self.G([bt1, bt2], [bq], lambda: nc.gpsimd.tensor_tensor(
            out=dst, in0=t1[0:64, 0:n].rearrange("p (a b) -> p a b", b=128), in1=t2[0:64, 0:n].rearrange("p (a b) -> p a b", b=128), op=ALU.add))

    def hy_filter(self, es, l, Lq, embname, tlname, width, Gd, bG, skipc, bskip, fcols, bfc, w1, w2, w3, bw):
        nc = self.nc
        ncol = 2 * Lq - 1
        ngrp = width // 512
        with ExitStack() as fs:
            nd = self.sbc(fs, "nd", [128, 4], F32)
            be = Buf()
            self.ld([], [be], nd[:], self.cin("ndelta").ap())
            h2 = self.sbc(fs, "h2", [64, width], BF16); bh2 = Buf()
            emb_r = Rot(nc, fs, "emb", [33, 512], F32, 2)
            tl_r = Rot(nc, fs, "tl", [128, 512], F32, 2)
            a_r = Rot(nc, fs, "farg", [64, 512], F32, 2)
            h1_r = Rot(nc, fs, "fh1", [64, 512], F32, 2)
            rr = (Rot(nc, fs, "rrt", [64, 512], F32, 2), Rot(nc, fs, "rri", [64, 512], mybir.dt.int32, 2))
            for gidx in range(ngrp):
                cs = slice(gidx * 512, (gidx + 1) * 512)
                emb, bemb = emb_r.next()
                self.ld([], [bemb], emb[:], self.cin(embname).ap()[:, cs])
                self.T([bemb, bw], [self.bpb[0]], lambda: nc.tensor.matmul(self.pb[0][0:64, :], lhsT=w1[:, :], rhs=emb[:, :], start=True, stop=True))
                a, ba = a_r.next()
                self.V([self.bpb[0], bfc], [ba], lambda: nc.vector.tensor_scalar(
                    out=a[:], in0=self.pb[0][0:64, :], scalar1=fcols[0:64, 0, 0:1], scalar2=fcols[0:64, 0, 1:2], op0=ALU.mult, op1=ALU.add))
                self.range_reduce(a, ba, rr)
                h1, bh1 = h1_r.next()
                self.A([ba], [bh1], lambda: nc.scalar.activation(out=h1[:], in_=a[:], func=AF.Sin))
                self.T([bh1, bw], [self.bpb[1]], lambda: nc.tensor.matmul(self.pb[1][0:64, :], lhsT=w2[:, :], rhs=h1[:], start=True, stop=True))
                a, ba = a_r.next()
                self.V([self.bpb[1], bfc], [ba], lambda: nc.vector.tensor_scalar(
                    out=a[:], in0=self.pb[1][0:64, :], scalar1=fcols[0:64, 0, 2:3], scalar2=fcols[0:64, 0, 3:4], op0=ALU.mult, op1=ALU.add))
                self.range_reduce(a, ba, rr)
                self.A([ba], [bh2], lambda: nc.scalar.activation(out=h2[:, cs], in_=a[:], func=AF.Sin))
            segs = []
            c0 = 0
            while c0 < ncol:
                lim = Lq if c0 < Lq else ncol
                n = min(512, lim - c0)
                segs.append((c0, n, c0 < Lq))
                c0 += n
            assert len(segs) <= 16
            graw = self.sbc(fs, "graw", [128, width], F32); bgr = Buf()
            dec_r = Rot(nc, fs, "dec", [128, 512], F32, 2)
            l1p = self.sbc(fs, "l1p", [128, 20], F32); bl1 = Buf()
            junk = self.sbc(fs, "fjunk", [128, 512], F32); bj = Buf()
            gb = self.sbc(fs, "gbf", [128, width], BF16); bgb = Buf()
            for i in range(4):
                self.V([], [bl1], lambda: nc.vector.memset(l1p[:], 0.0))
                for si, (c0, n, fwd) in enumerate(segs):
                    wc = (0 if fwd else 512) + i * 128
                    bank = 2 + si % 2
                    self.T([bh2, bw], [self.bpb[bank]], lambda: nc.tensor.matmul(
                        self.pb[bank][:, 0:n], lhsT=w3[:, wc:wc + 128], rhs=h2[:, c0:c0 + n], start=True, stop=True))
                    tl, btl = tl_r.next()
                    self.ld([], [btl], tl[:, 0:n], self.bc_ap(self.cin(tlname), c0, n))
                    dec, bdec = dec_r.next()
                    self.A([btl, be], [bdec], lambda: nc.scalar.activation(out=dec[:, 0:n], in_=tl[:, 0:n], func=AF.Exp, scale=nd[:, i:i + 1]))
                    self.V([self.bpb[bank], bdec], [bgr], lambda: nc.vector.tensor_tensor(
                        out=graw[:, c0:c0 + n], in0=self.pb[bank][:, 0:n], in1=dec[:, 0:n], op=ALU.mult))
                    self.A([bgr], [bj, bl1], lambda: nc.scalar.activation(out=junk[:, 0:n], in_=graw[:, c0:c0 + n], func=AF.Abs, accum_out=l1p[:, si:si + 1]))
                self.V([bl1], [bl1], lambda: nc.vector.reduce_sum(out=l1p[:, 16:17], in_=l1p[:, 0:16], axis=mybir.AxisListType.X))
                self.V([bl1], [bl1], lambda: nc.vector.reciprocal(out=l1p[:, 17:18], in_=l1p[:, 16:17]))
                self.V([bgr, bl1], [bgr], lambda: nc.vector.tensor_scalar(out=graw[:, 0:ncol], in0=graw[:, 0:ncol], scalar1=l1p[:, 17:18], scalar2=None, op0=ALU.mult))
                self.V([bgr, bskip], [bgr], lambda: nc.vector.tensor_tensor(out=graw[:, Lq - 1:Lq], in0=graw[:, Lq - 1:Lq], in1=skipc[:, i, 0:1], op=ALU.add))
                self.A([bgr], [bgb], lambda: nc.scalar.copy(out=gb[:, 0:ncol], in_=graw[:, 0:ncol]))
                self.ld([bgb], [bG[i]], Gd.ap()[i * 128:(i + 1) * 128, 0:ncol], gb[:, 0:ncol])

    def range_reduce(self, a, ba, rr):
        nc = self.nc
        TWO_PI = 2.0 * math.pi
        t, bt_ = rr[0].next(); ti, bti = rr[1].next()
        self.V([ba], [bt_], lambda: nc.vector.tensor_scalar(out=t[:], in0=a[:], scalar1=1.0 / TWO_PI, scalar2=None, op0=ALU.mult))
        self.V([bt_], [bti], lambda: nc.vector.tensor_copy(out=ti[:], in_=t[:]))
        self.V([bti], [bt_], lambda: nc.vector.tensor_copy(out=t[:], in_=ti[:]))
        self.V([bt_, ba], [ba], lambda: nc.vector.scalar_tensor_tensor(out=a[:], in0=t[:], scalar=-TWO_PI, in1=a[:], op0=ALU.mult, op1=ALU.add))
        self.V([ba], [bt_], lambda: nc.vector.tensor_scalar(out=t[:], in0=a[:], scalar1=0.0, scalar2=TWO_PI, op0=ALU.is_lt, op1=ALU.mult))
        self.V([bt_, ba], [ba], lambda: nc.vector.tensor_tensor(out=a[:], in0=a[:], in1=t[:], op=ALU.add))
        self.V([ba], [ba], lambda: nc.vector.tensor_scalar(out=a[:], in0=a[:], scalar1=-math.pi, scalar2=None, op0=ALU.add))

    def phase_hy(self, l, last):
        nc = self.nc
        with ExitStack() as es:
            skipc, bskip = self.load_cols(es, "skipc", self.w("hy_skip").ap()[l], 1, 512)
            cwc, bcw = self.load_cols(es, "cwc", self.w("hy_conv_w").ap()[l], 3, 1536)
            cbc, bcb = self.load_cols(es, "cbc", self.w("hy_conv_b").ap()[l], 1, 1536)
            frow = self.sbc(es, "frow", [4, 64], F32); bfr = Buf()
            for i, nm in enumerate(("hy_freq1", "hy_b1", "hy_freq2", "hy_b2")):
                self.ld([], [bfr], frow[i:i + 1, :], self.w(nm).ap()[l])
            fcols = self.sbc(es, "fcols", [64, 1, 4], F32); bfc = Buf()
            self.T([bfr, self.b_const], [self.bpb[7]], lambda: nc.tensor.transpose(out=self.pb[7][0:64, 0:4], in_=frow[:, :], identity=self.ident32[0:4, 0:4]))
            self.V([self.bpb[7]], [bfc], lambda: nc.vector.tensor_copy(out=fcols[:, 0, :], in_=self.pb[7][0:64, 0:4]))
            self.V([bfc], [bfc], lambda: nc.vector.tensor_tensor(out=fcols[:, 0, 1:2], in0=fcols[:, 0, 1:2], in1=fcols[:, 0, 0:1], op=ALU.mult))
            self.V([bfc], [bfc], lambda: nc.vector.tensor_tensor(out=fcols[:, 0, 3:4], in0=fcols[:, 0, 3:4], in1=fcols[:, 0, 2:3], op=ALU.mult))
            self.V([bfc], [bfc], lambda: nc.vector.tensor_scalar(out=fcols[:, 0, 1:2], in0=fcols[:, 0, 1:2], scalar1=17.0 * math.pi, scalar2=None, op0=ALU.add))
            self.V([bfc], [bfc], lambda: nc.vector.tensor_scalar(out=fcols[:, 0, 3:4], in0=fcols[:, 0, 3:4], scalar1=17.0 * math.pi, scalar2=None, op0=ALU.add))
            w1 = self.sbc(es, "fw1", [33, 64], F32); w2 = self.sbc(es, "fw2", [64, 64], F32); w3 = self.sbc(es, "fw3", [64, 1024], BF16)
            bw = Buf()
            self.ld([], [bw], w1[:], self.w("hy_w1").ap()[l]); self.ld([], [bw], w2[:], self.w("hy_w2").ap()[l]); self.ldcast([], [bw], w3[:], self.w("hy_w3").ap()[l])
            self.hy_filter(es, l, L, "embL", "tlinL", 8192, self.GS, self.bGS, skipc, bskip, fcols, bfc, w1, w2, w3, bw)
            self.fw.barrier()
            if not last:
                self.hy_filter(es, l, LC, "embC", "tlinC", 512, self.GC, self.bGC, skipc, bskip, fcols, bfc, w1, w2, w3, bw)
                self.fw.barrier()
            Yc = self.sbc(es, "hYc", [128, 3, NTOK], BF16); bYc = Buf()
            zT = self.sbc(es, "hzT", [128, NTOK], BF16); bz = Buf()
            Z = self.sbc(es, "hZ", [128, 128, NT], BF16); bZ = Buf()
            ysb = self.sbc(es, "hysb", [128, NT, 128], BF16); bys = Buf()
            ho_r = Rot(nc, es, "ho", [128, 512], BF16, 2)
            LOFF, COFF = 1, L + 3
            segs = [(LOFF, 0, L)] + ([] if last else [(COFF, L, LC)])
            ntok = NTOK if not last else L
            ntile = ntok // 128
            for i in range(4):
                with ExitStack() as sa:
                    W3 = self.sbc(sa, "hW3", [128, 3, 8, 128], BF16); bW = Buf()
                    U = self.sbc(sa, "hU", [128, NTOK + 4], F32); bU = Buf()
                    yt_r = Rot(nc, sa, "hyt", [128, 512], F32, 2)
                    self.V([], [bU], lambda: nc.vector.memset(U[:], 0.0))
                    for s_ in range(3):
                        self.ldcast([], [bW], W3[:, s_, :, :], self.wslice("w_in", l, C_HU + s_ * 512 + i * 128, 128))
                    for s_ in range(3):
                        ch = s_ * 4 + i
                        for gi_, (tok0, n) in enumerate(self.tok_groups(not last)):
                            uoff = (LOFF if tok0 < L else COFF - L) + tok0
                            bank = gi_ % 2
                            self.proj(bank, W3[:, s_], bW, 0, 128, tok0, n)
                            cp = self.A if gi_ % 2 == 0 else self.V
                            eng = nc.scalar.copy if gi_ % 2 == 0 else nc.vector.tensor_copy
                            cp([self.bpb[bank]], [bU], lambda: eng(out=U[:, uoff:uoff + n], in_=self.pb[bank][:, 0:n]))
                        for (uo, t0, nseq) in segs:
                            for p0 in range(0, nseq, 512):
                                n = min(512, nseq - p0)
                                yt, byt = yt_r.next()
                                self.A([bU, bcw, bcb], [byt], lambda: nc.scalar.activation(
                                    out=yt[:, 0:n], in_=U[:, uo + p0:uo + p0 + n], func=AF.Identity, scale=cwc[:, ch, 1:2], bias=cbc[:, ch, 0:1]))
                                self.V([bU, byt, bcw], [byt], lambda: nc.vector.scalar_tensor_tensor(
                                    out=yt[:, 0:n], in0=U[:, uo + p0 - 1:uo + p0 - 1 + n], scalar=cwc[:, ch, 0:1], in1=yt[:, 0:n], op0=ALU.mult, op1=ALU.add))
                                self.V([bU, byt, bcw], [bYc], lambda: nc.vector.scalar_tensor_tensor(
                                    out=Yc[:, s_, t0 + p0:t0 + p0 + n], in0=U[:, uo + p0 + 1:uo + p0 + 1 + n], scalar=cwc[:, ch, 2:3], in1=yt[:, 0:n], op0=ALU.mult, op1=ALU.add))
                    self.G([bYc], [bz], lambda: nc.gpsimd.tensor_tensor(out=zT[:, 0:ntok], in0=Yc[:, 1, 0:ntok], in1=Yc[:, 2, 0:ntok], op=ALU.mult))
                    for t0 in range(0, ntile, 4):
                        nt = min(4, ntile - t0)
                        bank = 6 + (t0 // 4) % 2
                        pbb = self.pb[bank][:, :].bitcast(BF16)
                        for j in range(nt):
                            self.T([bz, self.b_const], [self.bpb[bank]], lambda: nc.tensor.transpose(
                                out=pbb[:, j * 128:(j + 1) * 128], in_=zT[:, (t0 + j) * 128:(t0 + j + 1) * 128], identity=self.identb[:]))
                        self.V([self.bpb[bank]], [bZ], lambda: nc.vector.tensor_copy(
                            out=Z[:, :, t0:t0 + nt].rearrange("p c t -> p t c"), in_=pbb[:, 0:nt * 128].rearrange("p (a b) -> p a b", b=128)))
                    self.fw.barrier()
                with ExitStack() as sb_:
                    tr = Rot(nc, sb_, "hT", [128, 8064], BF16, 3)
                    tcx = self.sbc(sb_, "hTc", [128, 16, 384], BF16); btc = Buf()
                    order = [31] + [j for j in range(63) if j != 31]
                    for c in range(128):
                        Tt, bT = tr.next()
                        c_abs = i * 128 + c
                        self.ld([self.bGS[i]], [bT], Tt[:], bass.AP(tensor=self.GS, offset=c_abs * 8192, ap=[[1, 128], [1, 8064]]))
                        bank = (c // 16) % 2
                        col0 = (c % 16) * 32
                        for idx, j in enumerate(order):
                            d = 31 - j
                            s_lo, s_hi = (0, 32 - d) if d >= 0 else (-d, 32)
                            self.T([bT, bZ], [self.bpb[bank]], lambda: nc.tensor.matmul(
                                self.pb[bank][:, col0 + s_lo + d:col0 + s_hi + d], lhsT=Tt[:, j * 128:(j + 1) * 128], rhs=Z[:, c, s_lo:s_hi],
                                start=(idx == 0), stop=(idx == 62)))
                        if c % 16 == 15:
                            cb = c - 15
                            cp = self.A if (c // 16) % 2 == 0 else self.V
                            eng = nc.scalar.copy if (c // 16) % 2 == 0 else nc.vector.tensor_copy
                            cp([self.bpb[bank]], [bys], lambda: eng(
                                out=ysb[:, 0:32, cb:cb + 16].rearrange("p t c -> p c t"), in_=self.pb[bank][:, :].rearrange("p (c t) -> p c t", t=32)))
                    if not last:
                        for c16 in range(8):
                            c_abs = i * 128 + c16 * 16
                            self.ld([self.bGC[i]], [btc], tcx[:], bass.AP(tensor=self.GC, offset=c_abs * 512, ap=[[1, 128], [512, 16], [1, 384]]))
                            bank = 2 + c16 % 2
                            for cc in range(16):
                                c = c16 * 16 + cc
                                for idx, j in enumerate([1, 0, 2]):
                                    d = 1 - j
                                    s_lo, s_hi = (0, 2 - d) if d >= 0 else (-d, 2)
                                    self.T([btc, bZ], [self.bpb[bank]], lambda: nc.tensor.matmul(
                                        self.pb[bank][:, cc * 2 + s_lo + d:cc * 2 + s_hi + d], lhsT=tcx[:, cc, j * 128:(j + 1) * 128], rhs=Z[:, c, 32 + s_lo:32 + s_hi],
                                        start=(idx == 0), stop=(idx == 2)))
                            self.V([self.bpb[bank]], [bys], lambda: nc.vector.tensor_copy(
                                out=ysb[:, 32:34, c16 * 16:(c16 + 1) * 16].rearrange("p t c -> p c t"), in_=self.pb[bank][:, 0:32].rearrange("p (c t) -> p c t", t=2)))
                    for (tok0, n) in self.tok_groups(not last):
                        nt = n // 128
                        bank = 4 + (tok0 // 512) % 2
                        for j in range(nt):
                            self.T([bys, self.b_const], [self.bpb[bank]], lambda: nc.tensor.matmul(
                                self.pb[bank][:, j * 128:(j + 1) * 128], lhsT=ysb[:, tok0 // 128 + j, :], rhs=self.antib[:], start=True, stop=True))
                        ho, bho = ho_r.next()
                        self.V([self.bpb[bank], bYc], [bho], lambda: nc.vector.tensor_tensor(out=ho[:, 0:n], in0=self.pb[bank][:, 0:n], in1=Yc[:, 0, tok0:tok0 + n], op=ALU.mult))
                        self.ld([bho], [self.bMIX[2][i]], self.MIX.ap()[2, i * 128:(i + 1) * 128, tok0:tok0 + n], ho[:, 0:n])
                    self.fw.barrier()

    def phase_merge(self, l, last):
        nc = self.nc
        with ExitStack() as es:
            Wg = self.sbc(es, "mWg", [128, 8, 3072], BF16); Wb = self.sbc(es, "mWb", [128, 12, D], BF16); Wo = self.sbc(es, "mWo", [128, 8, D], BF16)
            bW = Buf()
            for cg in range(3):
                self.ldcast([], [bW], Wg[:, :, cg * 1024:(cg + 1) * 1024], self.wslice("w_in", l, C_MG + cg * 1024, 1024))
            self.ldcast([], [bW], Wb[:], self.w("w_branch").ap()[l].rearrange("(k p) n -> p k n", p=128))
            self.ldcast([], [bW], Wo[:], self.w("w_out").ap()[l].rearrange("(k p) n -> p k n", p=128))
            with ExitStack() as tmp:
                bgc, bbg = self.load_cols(es, "bgc", self.w("b_gate").ap()[l], 1, 3072, rows_es=tmp)
                self.fw.barrier()
            g1 = [self.sbc(es, f"g1_{r}", [128, D], F32) for r in range(2)]; bg1 = Buf()
            for r in range(2):
                self.ld([self.bMODV], [bg1], g1[r][:], self.bc_ap(self.MODV, r * 6 * D + 2 * D, D))
            MG = 512
            mix_r = Rot(nc, es, "mix", [128, 12, MG], BF16, 1)
            gs_r = Rot(nc, es, "gsb", [128, MG], F32, 3)
            macc = self.sbc(es, "macc", [128, MG], F32); bmacc = Buf()
            mtmp_r = Rot(nc, es, "mtmp", [128, MG], F32, 2)
            mT_r = Rot(nc, es, "mT", [128, 8, MG], BF16, 1)
            x_r = Rot(nc, es, "mx", [128, D], F32, 1)
            xo_r = Rot(nc, es, "mxo", [128, D], F32, 1)
            cnt = 0
            for (tok0, n) in self.tok_groups(not last):
                r = 0 if tok0 < L else 1
                mix, bmix = mix_r.next()
                self.ld([b for br in self.bMIX for b in br], [bmix], mix[:, :, 0:n],
                        self.MIX.ap()[:, :, tok0:tok0 + n].rearrange("b (k p) n -> p (b k) n", p=128))
                mT, bmT = mT_r.next()
                for oc in range(8):
                    for br in range(3):
                        bank_g = cnt % 3; bank_b = 3 + cnt % 3; cnt += 1
                        self.proj(bank_g, Wg, bW, br * 1024 + oc * 128, 128, tok0, n)
                        gsb, bgs = gs_r.next()
                        self.A([self.bpb[bank_g], bbg], [bgs], lambda: nc.scalar.activation(
                            out=gsb[:, 0:n], in_=self.pb[bank_g][:, 0:n], func=AF.Sigmoid, bias=bgc[:, br * 8 + oc, 0:1]))
                        for kc in range(4):
                            self.T([bW, bmix], [self.bpb[bank_b]], lambda: nc.tensor.matmul(
                                self.pb[bank_b][:, 0:n], lhsT=Wb[:, br * 4 + kc, oc * 128:(oc + 1) * 128], rhs=mix[:, br * 4 + kc, 0:n],
                                start=(kc == 0), stop=(kc == 3)))
                        if br == 0:
                            self.V([self.bpb[bank_b], bgs], [bmacc], lambda: nc.vector.tensor_tensor(out=macc[:, 0:n], in0=self.pb[bank_b][:, 0:n], in1=gsb[:, 0:n], op=ALU.mult))
                        else:
                            mt, bmt = mtmp_r.next()
                            self.V([self.bpb[bank_b], bgs], [bmt], lambda: nc.vector.tensor_tensor(out=mt[:, 0:n], in0=self.pb[bank_b][:, 0:n], in1=gsb[:, 0:n], op=ALU.mult))
                            if br == 1:
                                self.G([bmt, bmacc], [bmacc], lambda: nc.gpsimd.tensor_tensor(out=macc[:, 0:n], in0=macc[:, 0:n], in1=mt[:, 0:n], op=ALU.add))
                            else:
                                self.G([bmt, bmacc], [bmT], lambda: nc.gpsimd.tensor_tensor(out=mT[:, oc, 0:n], in0=macc[:, 0:n], in1=mt[:, 0:n], op=ALU.add))
                for j in range(n // 128):
                    t = tok0 // 128 + j
                    xt, bx = x_r.next()
                    self.ld([self.bXS[t]], [bx], xt[:], self.XS.ap()[t * 128:(t + 1) * 128, :])
                    xo, bxo = xo_r.next()
                    for half in range(2):
                        bank = 6 + half
                        for oc in range(8):
                            self.T([bmT, bW], [self.bpb[bank]], lambda: nc.tensor.matmul(
                                self.pb[bank][:, :], lhsT=mT[:, oc, j * 128:(j + 1) * 128], rhs=Wo[:, oc, half * 512:(half + 1) * 512],
                                start=(oc == 0), stop=(oc == 7)))
                        self.V([self.bpb[bank], bg1], [bxo], lambda: nc.vector.tensor_tensor(
                            out=xo[:, half * 512:(half + 1) * 512], in0=self.pb[bank][:, :], in1=g1[r][:, half * 512:(half + 1) * 512], op=ALU.mult))
                    self.G([bxo, bx], [bxo], lambda: nc.gpsimd.tensor_tensor(out=xo[:], in0=xo[:], in1=xt[:], op=ALU.add))
                    self.ld([bxo], [self.bXS[t]], self.XS.ap()[t * 128:(t + 1) * 128, :], xo[:])

    def phase_moe(self, l, last):
        nc = self.nc
        TG = 1024
        with ExitStack() as es:
            tmp = ExitStack()
            Al, Sl, bt = self.mod_tables(es, l, "norm2_g", 3 * D, 4 * D, "n2")
            g2 = [self.sbc(es, f"g2_{r}", [128, D], F32) for r in range(2)]; bg2 = Buf()
            for r in range(2):
                self.ld([self.bMODV], [bg2], g2[r][:], self.bc_ap(self.MODV, r * 6 * D + 5 * D, D))
            fng = None
            if last:
                fng = self.sbc(es, "fng", [128, D], F32)
                self.ld([], [bg2], fng[:], self.bc_ap(self.w("final_norm_g"), 0, D))
            b1c, bb1 = self.load_cols(es, "b1c", self.w("moe_b1").ap()[l], 32, 2048, bank=7, rows_es=tmp)
            self.fw.barrier()
            tmp.close()
            self.V([bb1], [bb1], lambda: nc.vector.tensor_scalar(out=b1c[:, 8:16, :], in0=b1c[:, 8:16, :], scalar1=1.0, scalar2=None, op0=ALU.add))
            b2s = self.sbc(es, "b2s", [32, D], F32); bb2 = Buf()
            self.ld([], [bb2], b2s[:], self.w("moe_b2").ap()[l])
            Wr = self.sbc(es, "Wr", [128, 8, NEXP], F32); bWr = Buf()
            self.ld([], [bWr], Wr[:], self.w("router_w").ap()[l].rearrange("(k p) n -> p k n", p=128))
            rb = self.sbc(es, "rb", [128, NEXP], F32)
            self.ld([], [bWr], rb[:], self.bc_ap(self.w("router_b"), l * NEXP, NEXP))
            h2T = self.sbc(es, "h2T", [128, 8, TG], BF16); bh2 = Buf()
            h32_r = Rot(nc, es, "h32", [128, 8, 128], F32, 1)
            comb = self.sbc(es, "comb", [128, 8, NEXP], F32); bcomb = Buf()
            combs = self.sbc(es, "combs", [128, 8, NEXP], F32)
            combT = self.sbc(es, "combT", [32, 8, 128], F32); bcT = Buf()
            yacc = self.sbc(es, "yacc", [128, 8, D], F32); byacc = [Buf() for _ in range(8)]
            actT = self.sbc(es, "actT", [128, 8, TG], BF16); bact = [Buf() for _ in range(2)]
            W1r = Rot(nc, es, "W1", [128, 8, 2 * DFF], BF16, 1)
            W2r = Rot(nc, es, "W2", [128, 8, D], BF16, 2)
            xr = Rot(nc, es, "ex", [128, D], F32, 1)
            xnr = Rot(nc, es, "exn", [128, D], F32, 1)
            junk = self.sbc(es, "ejunk", [128, D], F32); bj = Buf()
            str_ = Rot(nc, es, "est", [128, 4], F32, 2)
            lg_r = Rot(nc, es, "elg", [128, 48], F32, 2)
            g_r = Rot(nc, es, "eg", [128, 512], F32, 2)
            s_r = Rot(nc, es, "es", [128, 512], BF16, 2)
            l_r = Rot(nc, es, "el", [128, 512], F32, 2)
            groups = [(i * TG, TG) for i in range(L // TG)] + ([] if last else [(L, LC)])
            for (tok0, n) in groups:
                r = 0 if tok0 < L else 1
                ntile = n // 128
                for j in range(ntile):
                    t = tok0 // 128 + j
                    xt, bx = xr.next(); xn, bxn = xnr.next(); st, bst = str_.next()
                    self.ld([self.bXS[t]], [bx], xt[:], self.XS.ap()[t * 128:(t + 1) * 128, :])
                    self.norm_tile(xt[:], bx, xn[:], bxn, Al[r], Sl[r], bt, (junk, bj, st, bst), add_on_dve=True)
                    h32, bh32 = h32_r.next()
                    for half in range(2):
                        bank = half
                        for kk in range(4):
                            k = half * 4 + kk
                            self.T([bxn, self.b_const], [self.bpb[bank]], lambda: nc.tensor.transpose(
                                out=self.pb[bank][:, kk * 128:(kk + 1) * 128], in_=xn[:, k * 128:(k + 1) * 128], identity=self.ident32[:]))
                        src = self.pb[bank][:, :].rearrange("p (a b) -> p a b", a=4)
                        self.A([self.bpb[bank]], [bh2], lambda: nc.scalar.copy(out=h2T[:, half * 4:half * 4 + 4, j * 128:(j + 1) * 128], in_=src))
                        self.V([self.bpb[bank]], [bh32], lambda: nc.vector.tensor_copy(out=h32[:, half * 4:half * 4 + 4, :], in_=src))
                    for k in range(8):
                        self.T([bh32, bWr], [self.bpb[2]], lambda: nc.tensor.matmul(
                            self.pb[2][:, 0:NEXP], lhsT=h32[:, k, :], rhs=Wr[:, k, :], start=(k == 0), stop=(k == 7)))
                    lg, blg = lg_r.next()
                    self.V([self.bpb[2], bWr], [blg], lambda: nc.vector.tensor_tensor(out=lg[:, 0:32], in0=self.pb[2][:, 0:NEXP], in1=rb[:], op=ALU.add))
                    self.V([blg], [blg], lambda: nc.vector.max(out=lg[:, 32:40], in_=lg[:, 0:32]))
                    self.V([blg], [blg], lambda: nc.vector.tensor_scalar(out=lg[:, 40:41], in0=lg[:, 32:33], scalar1=-1.0, scalar2=None, op0=ALU.mult))
                    self.V([blg], [bcomb], lambda: nc.vector.tensor_scalar(out=combs[:, j, :], in0=lg[:, 0:32], scalar1=lg[:, 35:36], scalar2=None, op0=ALU.is_ge))
                    self.A([blg], [blg], lambda: nc.scalar.activation(out=lg[:, 0:32], in_=lg[:, 0:32], func=AF.Exp, bias=lg[:, 40:41]))
                    self.V([blg, bcomb], [bcomb], lambda: nc.vector.tensor_tensor(out=combs[:, j, :], in0=combs[:, j, :], in1=lg[:, 0:32], op=ALU.mult))
                    self.V([bcomb], [blg], lambda: nc.vector.reduce_sum(out=lg[:, 41:42], in_=combs[:, j, :], axis=mybir.AxisListType.X))
                    self.V([blg], [blg], lambda: nc.vector.reciprocal(out=lg[:, 42:43], in_=lg[:, 41:42]))
                    self.V([blg, bcomb], [bcomb], lambda: nc.vector.tensor_scalar(out=comb[:, j, :], in0=combs[:, j, :], scalar1=lg[:, 42:43], scalar2=None, op0=ALU.mult))
                    self.V([bcomb], [bcomb], lambda: nc.vector.tensor_scalar(out=combs[:, j, :], in0=comb[:, j, :], scalar1=1.0 / 1.702, scalar2=None, op0=ALU.mult))
                    self.T([bcomb, self.b_const], [self.bpb[3]], lambda: nc.tensor.transpose(out=self.pb[3][0:32, 0:128], in_=comb[:, j, :], identity=self.ident32[:]))
                    self.V([self.bpb[3]], [bcT], lambda: nc.vector.tensor_copy(out=combT[:, j, :], in_=self.pb[3][0:32, 0:128]))
                    for half in range(2):
                        bank = 4 + half
                        self.T([bcT, bb2], [self.bpb[bank]], lambda: nc.tensor.matmul(
                            self.pb[bank][:, :], lhsT=combT[:, j, :], rhs=b2s[:, half * 512:(half + 1) * 512], start=True, stop=True))
                        self.A([self.bpb[bank]], [byacc[j]], lambda: nc.scalar.copy(out=yacc[:, j, half * 512:(half + 1) * 512], in_=self.pb[bank][:, :]))
                ntg = (n + 511) // 512
                for e in range(NEXP):
                    W1, bW1 = W1r.next(); W2, bW2 = W2r.next()
                    self.ldcast([], [bW1], W1[:], self.w("moe_w1").ap()[l, e].rearrange("(k p) n -> p k n", p=128))
                    self.ldcast([], [bW2], W2[:], self.w("moe_w2").ap()[l, e].rearrange("(k p) n -> p k n", p=128))
                    cnt = 0
                    for tg in range(ntg):
                        t0 = tg * 512; m = min(512, n - t0)
                        for fc in range(8):
                            bg_, bl_ = (0, 1) if cnt % 2 == 0 else (2, 3)
                            cnt += 1
                            for k in range(8):
                                self.T([bW1, bh2], [self.bpb[bg_]], lambda: nc.tensor.matmul(
                                    self.pb[bg_][:, 0:m], lhsT=W1[:, k, fc * 128:(fc + 1) * 128], rhs=h2T[:, k, t0:t0 + m], start=(k == 0), stop=(k == 7)))
                            for k in range(8):
                                self.T([bW1, bh2], [self.bpb[bl_]], lambda: nc.tensor.matmul(
                                    self.pb[bl_][:, 0:m], lhsT=W1[:, k, DFF + fc * 128:DFF + (fc + 1) * 128], rhs=h2T[:, k, t0:t0 + m], start=(k == 0), stop=(k == 7)))
                            gg, bgg = g_r.next(); ss, bss = s_r.next(); ll, bll = l_r.next()
                            self.V([self.bpb[bg_], bb1], [bgg], lambda: nc.vector.tensor_scalar(
                                out=gg[:, 0:m], in0=self.pb[bg_][:, 0:m], scalar1=b1c[:, fc, e:e + 1], scalar2=7.0, op0=ALU.add, op1=ALU.min))
                            self.A([bgg], [bss], lambda: nc.scalar.activation(out=ss[:, 0:m], in_=gg[:, 0:m], func=AF.Silu, scale=1.702))
                            self.V([self.bpb[bl_], bb1], [bll], lambda: nc.vector.tensor_scalar(
                                out=ll[:, 0:m], in0=self.pb[bl_][:, 0:m], scalar1=b1c[:, 8 + fc, e:e + 1], scalar2=8.0, op0=ALU.add, op1=ALU.min))
                            self.V([bll, bss], [bact[tg]], lambda: nc.vector.scalar_tensor_tensor(
                                out=actT[:, fc, t0:t0 + m], in0=ll[:, 0:m], scalar=-6.0, in1=ss[:, 0:m], op0=ALU.max, op1=ALU.mult))
                    for j in range(ntile):
                        for half in range(2):
                            bank = 4 + (2 * j + half) % 4
                            for fc in range(8):
                                self.T([bact[j // 4], bW2], [self.bpb[bank]], lambda: nc.tensor.matmul(
                                    self.pb[bank][:, :], lhsT=actT[:, fc, j * 128:(j + 1) * 128], rhs=W2[:, fc, half * 512:(half + 1) * 512],
                                    start=(fc == 0), stop=(fc == 7)))
                            ya = yacc[:, j, half * 512:(half + 1) * 512]
                            self.V([self.bpb[bank], bcomb, byacc[j]], [byacc[j]], lambda: nc.vector.scalar_tensor_tensor(
                                out=ya, in0=self.pb[bank][:, :], scalar=combs[:, j, e:e + 1], in1=ya, op0=ALU.mult, op1=ALU.add))
                for j in range(ntile):
                    t = tok0 // 128 + j
                    xt, bx = xr.next(); xn, bxn = xnr.next()
                    self.ld([self.bXS[t]], [bx], xt[:], self.XS.ap()[t * 128:(t + 1) * 128, :])
                    self.V([byacc[j], bg2], [byacc[j]], lambda: nc.vector.tensor_tensor(out=yacc[:, j, :], in0=yacc[:, j, :], in1=g2[r][:], op=ALU.mult))
                    self.V([byacc[j], bx], [bxn], lambda: nc.vector.tensor_tensor(out=xn[:], in0=yacc[:, j, :], in1=xt[:], op=ALU.add))
                    if not last:
                        self.ld([bxn], [self.bXS[t]], self.XS.ap()[t * 128:(t + 1) * 128, :], xn[:])
                    else:
                        st, bst = str_.next()
                        self.V([], [bst], lambda: nc.vector.memset(st[:, 0:1], 0.0))
                        self.A([bxn], [bj, bst], lambda: nc.scalar.activation(out=junk[:], in_=xn[:], func=AF.Square, accum_out=st[:, 0:1]))
                        self.V([bst], [bst], lambda: nc.vector.tensor_scalar(out=st[:, 1:2], in0=st[:, 0:1], scalar1=1.0 / D, scalar2=EPS, op0=ALU.mult, op1=ALU.add))
                        self.A([bst], [bst], lambda: nc.scalar.activation(out=st[:, 2:3], in_=st[:, 1:2], func=AF.Sqrt))
                        self.V([bst], [bst], lambda: nc.vector.reciprocal(out=st[:, 3:4], in_=st[:, 2:3]))
                        self.V([bxn, bst, bg2], [bx], lambda: nc.vector.scalar_tensor_tensor(
                            out=xt[:], in0=xn[:], scalar=st[:, 3:4], in1=fng[:], op0=ALU.mult, op1=ALU.mult))
                        self.ld([bx], [self.bOUT], self.OUT.ap()[t * 128:(t + 1) * 128, :], xt[:])


def make_in_maps(prog, inputs):
    consts = host_consts()
    n = 8
    shared = {}
    reshapes = dict(ret_decay_logit=(DEPTH, 8), hy_conv_b=(DEPTH, 1, 1536), hy_b1=(DEPTH, 1, 64), hy_freq1=(DEPTH, 1, 64),
                    hy_b2=(DEPTH, 1, 64), hy_freq2=(DEPTH, 1, 64), hy_skip=(DEPTH, 1, 512), w_branch=(DEPTH, 1536, D),
                    b_gate=(DEPTH, 1, 3072), final_norm_g=(1, D))
    for name in prog.din:
        if name.startswith("k_"):
            shared[name] = np.ascontiguousarray(consts[name[2:]])
        elif name in ("x", "ctx", "cc"):
            continue
        else:
            a = np.asarray(inputs[name], dtype=np.float32)
            if name in reshapes:
                a = a.reshape(reshapes[name])
            shared[name] = np.ascontiguousarray(a)
    maps = []
    for b in range(n):
        m = dict(shared)
        if "x" in prog.din:
            m["x"] = np.ascontiguousarray(inputs["x"][b], dtype=np.float32)
        if "ctx" in prog.din:
            m["ctx"] = np.ascontiguousarray(inputs["ctx"][b], dtype=np.float32)
        if "cc" in prog.din:
            m["cc"] = np.ascontiguousarray(np.concatenate([np.asarray(inputs["c"][b], np.float32).reshape(8, 128),
                                                           np.asarray(inputs["c_ctx"], np.float32).reshape(8, 128)], 0))
        maps.append(m)
    return maps


def kernel(**inputs):
    prog = Prog()
    nc = prog.build()
    maps = make_in_maps(prog, inputs)
    res = run_bass_kernel_spmd(nc, maps, core_ids=list(range(8)))
    return np.stack([np.asarray(r["OUT"], dtype=np.float32) for r in res.results], 0)
```

```python
import math
from contextlib import ExitStack

import numpy as np
import ml_dtypes
import concourse.bass as bass
import concourse.mybir as mybir
from concourse.bass_utils import run_bass_kernel_spmd

F32 = mybir.dt.float32
BF16 = mybir.dt.bfloat16
AF = mybir.ActivationFunctionType
ALU = mybir.AluOpType

D = 1024
L = 4096
LC = 256
NT = 34
NTOK = NT * 128
DEPTH = 2
EPS = 1e-6
IN_COLS = 7424
C_RQ, C_RK, C_RV, C_RG, C_AQ, C_AK, C_AV, C_HU, C_MG = 0, 512, 1024, 1536, 2048, 2560, 2688, 2816, 4352
NEXP = 32
DFF = 1024


class Truncate(Exception):
    pass


class Buf:
    __slots__ = ("w", "r", "x")

    def __init__(self, excl=False):
        self.w = {}
        self.r = {}
        self.x = excl


class Eng:
    def __init__(self, name, eng, sem, is_pe=False):
        self.name, self.eng, self.sem, self.is_pe = name, eng, sem, is_pe
        self.count = 0
        self.seen = {}


class FW:
    NDMA = 32

    def __init__(self, nc):
        self.nc = nc
        self.es = ExitStack()
        mk = lambda n: self.es.enter_context(nc.semaphore(n))
        self.pe = Eng("pe", nc.tensor, mk("s_pe"), True)
        self.act = Eng("act", nc.scalar, mk("s_act"))
        self.dve = Eng("dve", nc.vector, mk("s_dve"))
        self.pool = Eng("pool", nc.gpsimd, mk("s_pool"))
        self.sp = Eng("sp", nc.sync, mk("s_sp"))
        self.engs = [self.pe, self.act, self.dve, self.pool, self.sp]
        self.dsems = [mk(f"s_d{i}") for i in range(self.NDMA)]
        self.dval = [0] * self.NDMA
        self.dnext = 0
        self.dnext_sw = 0
        self.semof = {e.name: e.sem for e in self.engs}
        for i in range(self.NDMA):
            self.semof[("d", i)] = self.dsems[i]
        self.ninst = 0

    def _need(self, E, reads, writes):
        deps = {}
        for b in reads:
            for k, v in b.w.items():
                if deps.get(k, 0) < v:
                    deps[k] = v
            if b.x:
                for k, v in b.r.items():
                    if k != E.name and deps.get(k, 0) < v:
                        deps[k] = v
        for b in writes:
            for k, v in b.w.items():
                if deps.get(k, 0) < v:
                    deps[k] = v
            for k, v in b.r.items():
                if deps.get(k, 0) < v:
                    deps[k] = v
        for k, v in deps.items():
            if k == E.name and E.is_pe:
                continue
            if E.seen.get(k, 0) >= v:
                continue
            E.eng.wait_ge(self.semof[k], v)
            E.seen[k] = v

    max_ops = None

    def op(self, E, reads, writes, fn):
        if self.max_ops is not None and self.ninst >= self.max_ops:
            raise Truncate()
        self._need(E, reads, writes)
        ins = fn()
        ins.then_inc(E.sem, 1)
        E.count += 1
        self.ninst += 1
        c = E.count
        for b in reads:
            b.r[E.name] = c
        for b in writes:
            b.w = {E.name: c}
            b.r = {}
        return ins

    def dma(self, Q, reads, writes, out, in_, **kw):
        half = self.NDMA // 2
        if Q is self.pool:
            j = half + self.dnext_sw
            self.dnext_sw = (self.dnext_sw + 1) % half
        else:
            j = self.dnext
            self.dnext = (self.dnext + 1) % half
        key = ("d", j)
        if self.dval[j] and Q.seen.get(key, 0) < self.dval[j]:
            Q.eng.wait_ge(self.dsems[j], self.dval[j])
            Q.seen[key] = self.dval[j]
        self._need(Q, reads, writes)
        ins = Q.eng.dma_start(out=out, in_=in_, **kw)
        ins.then_inc(self.dsems[j], 16)
        self.dval[j] += 16
        v = self.dval[j]
        self.ninst += 1
        for b in reads:
            b.r[key] = v
        for b in writes:
            b.w = {key: v}
            b.r = {}
        return ins

    def barrier(self):
        for E in self.engs:
            for F in self.engs:
                if F is E or F.count == 0:
                    continue
                if E.seen.get(F.name, 0) < F.count:
                    E.eng.wait_ge(F.sem, F.count)
                    E.seen[F.name] = F.count
            for j in range(self.NDMA):
                key = ("d", j)
                if self.dval[j] and E.seen.get(key, 0) < self.dval[j]:
                    E.eng.wait_ge(self.dsems[j], self.dval[j])
                    E.seen[key] = self.dval[j]


class Rot:
    _uid = 0

    def __init__(self, nc, es, name, shape, dt, n=2):
        Rot._uid += 1
        self.t = [es.enter_context(nc.sbuf_tensor(f"{name}{i}_r{Rot._uid}", shape, dt)) for i in range(n)]
        self.b = [Buf() for _ in range(n)]
        self.i = -1
        self.n = n

    def next(self):
        self.i = (self.i + 1) % self.n
        return self.t[self.i], self.b[self.i]


_CONST_CACHE = {}


def host_consts():
    if _CONST_CACHE:
        return _CONST_CACHE
    c = {}
    f32 = np.float32
    c["ident32"] = np.eye(128, dtype=f32)
    c["identb"] = np.eye(128).astype(ml_dtypes.bfloat16)
    c["antib"] = np.eye(128)[::-1].copy().astype(ml_dtypes.bfloat16)
    c["onesb"] = np.full((128, 128), 1.0 / 128.0).astype(ml_dtypes.bfloat16)
    inv_r = (1.0 / (10000.0 ** np.linspace(0.0, 1.0, 64, dtype=f32))).astype(f32)
    tpos = np.arange(L, dtype=f32)
    ang = tpos[None, :] * inv_r[:, None]
    c["cosR"] = np.concatenate([np.cos(ang), np.cos(ang)], 0).astype(f32)
    c["sinR"] = np.concatenate([np.sin(ang), np.sin(ang)], 0).astype(f32)
    pr = np.zeros((128, 128), f32)
    for dp in range(64):
        pr[dp + 64, dp] = -1.0
        pr[dp, dp + 64] = 1.0
    c["permR"] = pr.astype(ml_dtypes.bfloat16)
    inv_a = (1.0 / (10000.0 ** (np.arange(16, dtype=f32) / 16))).astype(f32)
    rows = np.repeat(np.arange(L // 64, dtype=f32), 64)
    cols = np.tile(np.arange(64, dtype=f32), L // 64)
    angr = rows[None, :] * inv_a[:, None]
    angc = cols[None, :] * inv_a[:, None]
    c["cosA"] = np.concatenate([np.cos(angr), np.cos(angr), np.cos(angc), np.cos(angc)], 0).astype(f32)
    c["sinA"] = np.concatenate([np.sin(angr), np.sin(angr), np.sin(angc), np.sin(angc)], 0).astype(f32)
    pa = np.zeros((64, 64), f32)
    for base in (0, 32):
        for dp in range(16):
            pa[base + dp + 16, base + dp] = -1.0
            pa[base + dp, base + dp + 16] = 1.0
    c["permA"] = pa.astype(ml_dtypes.bfloat16)
    j = np.arange(128, dtype=f32)[:, None]
    i = np.arange(128, dtype=f32)[None, :]
    ks = 128.0 ** -0.5
    rt = np.zeros((128, 6, 128), f32)
    rt[:, 0] = np.maximum(i - j, 0.0)
    rt[:, 1] = (j <= i) * ks
    rt[:, 2] = np.maximum(j - i, 0.0)
    rt[:, 3] = (j > i) * ks
    rt[:, 4] = i + 1.0
    rt[:, 5] = 128.0 - i
    c["rtab"] = rt
    rc = np.zeros((128, 4), f32)
    rc[:, 0] = 127.0 - np.arange(128)
    rc[:, 1] = np.arange(128)
    rc[:, 2] = 128.0
    c["rcol"] = rc
    am = np.zeros((128, 2, 128), f32)
    am[:, 0] = (j >= i)
    am[:, 1] = (j <= i)
    c["amask"] = am.astype(ml_dtypes.bfloat16)
    def emb(Lq, width):
        t = np.linspace(0.0, 1.0, Lq, dtype=f32)
        bands = np.linspace(1e-4, 15, 16, dtype=f32)
        ang = (f32(2.0 * math.pi / Lq) * np.arange(Lq, dtype=f32)[:, None] * bands[None, :]).astype(f32)
        z = np.concatenate([t[:, None], np.cos(ang), -np.sin(ang)], -1).astype(f32)
        tidx = np.concatenate([np.arange(Lq - 1, -1, -1), np.arange(1, Lq)])
        e = np.zeros((33, width), f32)
        e[:, :2 * Lq - 1] = z[tidx].T
        tl = np.zeros((1, width), f32)
        tl[0, :2 * Lq - 1] = t[tidx]
        return e, tl
    c["embL"], c["tlinL"] = emb(L, 8192)
    c["embC"], c["tlinC"] = emb(LC, 512)
    deltas = np.abs(np.linspace(math.log(1e-2) / 1.5, math.log(1e-2) / 0.3, 512, dtype=f32)).astype(f32)
    c["ndelta"] = np.ascontiguousarray((-deltas).reshape(4, 128).T)
    _CONST_CACHE.update(c)
    return c


class Prog:
    def __init__(self, layers=(0, 1), phases=None, debug=False):
        self.layers = layers
        self.phases = phases
        self.debug = debug
        nc = self.nc = bass.Bass("TRN2", target_bir_lowering=False)
        self.fw = FW(nc)
        self.din = {}
        self.consts = host_consts()
        ges = self.ges = self.fw.es
        self.pb = [ges.enter_context(nc.psum_tensor(f"pb{i}", [128, 512], F32)) for i in range(8)]
        self.bpb = [Buf(excl=True) for _ in range(8)]
        okind = "ExternalOutput" if debug else "Internal"
        self.XS = nc.dram_tensor("XS", [NTOK, D], F32, kind=okind)
        self.bXS = [Buf() for _ in range(NT)]
        self.MIX = nc.dram_tensor("MIX", [3, 512, NTOK], BF16, kind=okind)
        self.bMIX = [[Buf() for _ in range(4)] for _ in range(3)]
        self.MODV = nc.dram_tensor("MODV", [2, 6 * D], F32, kind=okind)
        self.bMODV = Buf()
        self.GS = nc.dram_tensor("GS", [512, 8192], BF16, kind=okind)
        self.GC = nc.dram_tensor("GC", [512, 512], BF16, kind=okind)
        self.bGS = [Buf() for _ in range(4)]
        self.bGC = [Buf() for _ in range(4)]
        self.OUT = nc.dram_tensor("OUT", [L, D], F32, kind="ExternalOutput")
        self.bOUT = Buf()
        self.ident32, self.b_const = self.sbc(ges, "ident32", [128, 128], F32), Buf()
        self.identb = self.sbc(ges, "identb", [128, 128], BF16)
        self.antib = self.sbc(ges, "antib", [128, 128], BF16)
        self.onesb = self.sbc(ges, "onesb", [128, 128], BF16)
        for nm, t in (("ident32", self.ident32), ("identb", self.identb), ("antib", self.antib), ("onesb", self.onesb)):
            self.fw.dma(self.fw.sp, [], [self.b_const], t[:], self.cin(nm).ap())
        self.h1T = None

    def sbc(self, es, name, shape, dt):
        self._uid = getattr(self, "_uid", 0) + 1
        return es.enter_context(self.nc.sbuf_tensor(f"{name}_u{self._uid}", shape, dt))

    def inp(self, name, shape, dt=F32):
        if name not in self.din:
            self.din[name] = self.nc.dram_tensor(name, list(shape), dt, kind="ExternalInput")
        return self.din[name]

    def cin(self, name):
        a = self.consts[name]
        dt = BF16 if a.dtype == ml_dtypes.bfloat16 else F32
        return self.inp("k_" + name, a.shape, dt)

    def w(self, name):
        shapes = dict(
            w_mod=[DEPTH, D, 6 * D], b_mod=[DEPTH, 6 * D], norm1_g=[DEPTH, D], w_in=[DEPTH, D, IN_COLS],
            ret_decay_logit=[DEPTH, 8], attn_sink=[DEPTH, 8], hy_conv_w=[DEPTH, 3, 1536], hy_conv_b=[DEPTH, 1, 1536],
            hy_w1=[DEPTH, 33, 64], hy_b1=[DEPTH, 1, 64], hy_freq1=[DEPTH, 1, 64], hy_w2=[DEPTH, 64, 64],
            hy_b2=[DEPTH, 1, 64], hy_freq2=[DEPTH, 1, 64], hy_w3=[DEPTH, 64, 1024], hy_skip=[DEPTH, 1, 512],
            w_branch=[DEPTH, 1536, D], b_gate=[DEPTH, 1, 3072], w_out=[DEPTH, D, D], norm2_g=[DEPTH, D],
            router_w=[DEPTH, D, NEXP], router_b=[DEPTH, NEXP], moe_w1=[DEPTH, NEXP, D, 2 * DFF],
            moe_b1=[DEPTH, NEXP, 2 * DFF], moe_w2=[DEPTH, NEXP, DFF, D], moe_b2=[DEPTH, NEXP, D],
            final_norm_g=[1, D], x=[L, D], ctx=[LC, D], cc=[16, 128])
        return self.inp(name, shapes[name])

    def bc_ap(self, t, off, n, parts=128):
        return bass.AP(tensor=t, offset=off, ap=[[0, parts], [1, n]])

    def V(self, r, w, f):
        return self.fw.op(self.fw.dve, r, w, f)

    def A(self, r, w, f):
        return self.fw.op(self.fw.act, r, w, f)

    def G(self, r, w, f):
        return self.fw.op(self.fw.pool, r, w, f)

    def T(self, r, w, f):
        return self.fw.op(self.fw.pe, r, w, f)

    def ld(self, r, w, out, in_, q=None):
        return self.fw.dma(q or self.fw.sp, r, w, out, in_)

    def ldcast(self, r, w, out, in_):
        return self.fw.dma(self.fw.pool, r, w, out, in_)

    def wslice(self, name, l, c0, n):
        t = self.w(name)
        return t.ap()[l, :, c0:c0 + n].rearrange("(k p) n -> p k n", p=128)

    def load_cols(self, es, name, src_rows_ap, R, N, bank=7, rows_es=None):
        nc = self.nc
        n = min(N, 128)
        nch = (N + 127) // 128
        dst = self.sbc(es, name, [128, nch, R], F32)
        bdst = Buf()
        rows = self.sbc(rows_es or es, name + "_r", [R, N], F32)
        brow = Buf()
        self.ld([], [brow], rows[:], src_rows_ap)
        for j in range(nch):
            self.T([brow, self.b_const], [self.bpb[bank]], lambda: nc.tensor.transpose(
                out=self.pb[bank][0:n, j * R:(j + 1) * R], in_=rows[0:R, j * 128:j * 128 + n], identity=self.ident32[0:R, 0:R]))
        self.V([self.bpb[bank]], [bdst], lambda: nc.vector.tensor_copy(
            out=dst[0:n, :, :], in_=self.pb[bank][0:n, 0:nch * R].rearrange("p (c r) -> p c r", r=R)))
        return dst, bdst

    def proj(self, bank, Wt, bW, c0, m, tok0, n, out_p0=0):
        nc = self.nc
        for k in range(8):
            self.T([bW, self.bh1T], [self.bpb[bank]], lambda: nc.tensor.matmul(
                self.pb[bank][out_p0:out_p0 + m, 0:n], lhsT=Wt[:, k, c0:c0 + m], rhs=self.h1T[:, k, tok0:tok0 + n],
                start=(k == 0), stop=(k == 7)))

    def run_phase(self, name):
        return self.phases is None or name in self.phases

    def build(self):
        nc, fw = self.nc, self.fw
        try:
            self._build_body()
        except Truncate:
            print("TRUNCATED at", fw.ninst)
        fw._need(fw.sp, [self.bOUT] + self.bXS, [])
        fw.barrier()
        return nc

    def _build_body(self):
        nc, fw = self.nc, self.fw
        if self.run_phase("init"):
            for t0 in range(0, 32, 8):
                self.ld([], self.bXS[t0:t0 + 8], self.XS.ap()[t0 * 128:(t0 + 8) * 128, :], self.w("x").ap()[t0 * 128:(t0 + 8) * 128, :])
            self.ld([], self.bXS[32:34], self.XS.ap()[L:L + LC, :], self.w("ctx").ap())
        for l in self.layers:
            last = (l == DEPTH - 1)
            if self.run_phase("mod"):
                self.phase_mod(l)
                fw.barrier()
            with ExitStack() as mes:
                if any(self.run_phase(p) for p in ("h1", "ret", "att", "hy", "merge")):
                    self.h1T = self.sbc(mes, "h1T", [128, 8, NTOK], BF16)
                    self.bh1T = Buf()
                    self.phase_norm_T(l)
                    fw.barrier()
                    if self.debug:
                        dbg = nc.dram_tensor(f"DBG_h1T{l}", [128, 8, NTOK], BF16, kind="ExternalOutput")
                        self.ld([self.bh1T], [Buf()], dbg.ap(), self.h1T[:])
                        fw.barrier()
                if self.run_phase("ret"):
                    self.phase_ret(l, last)
                    fw.barrier()
                if self.run_phase("att"):
                    self.phase_att(l, last)
                    fw.barrier()
                if self.run_phase("hy"):
                    self.phase_hy(l, last)
                    fw.barrier()
                if self.run_phase("merge"):
                    self.phase_merge(l, last)
                    fw.barrier()
            if self.run_phase("moe"):
                self.phase_moe(l, last)
                fw.barrier()

    def phase_mod(self, l):
        nc = self.nc
        with ExitStack() as es:
            crow = self.sbc(es, "crow", [16, 128], F32); bcrow = Buf()
            self.ld([], [bcrow], crow[:], self.w("cc").ap())
            cs = self.sbc(es, "cs", [128, 16], F32); bcs = Buf()
            self.T([bcrow, self.b_const], [self.bpb[0]], lambda: nc.tensor.transpose(
                out=self.pb[0][:, 0:16], in_=crow[:, :], identity=self.ident32[0:16, 0:16]))
            self.A([self.bpb[0]], [bcs], lambda: nc.scalar.activation(out=cs[:], in_=self.pb[0][:, 0:16], func=AF.Silu))
            bm = self.sbc(es, "bm", [2, 6 * D], F32); bbm = Buf()
            for r in range(2):
                self.ld([], [bbm], bm[r:r + 1, :], self.w("b_mod").ap()[l:l + 1, :])
            modv = self.sbc(es, "modv", [2, 6 * D], F32); bmodv = Buf()
            wr = Rot(nc, es, "wmod", [128, 8, 512], F32, 2)
            for cg in range(12):
                wt, bw = wr.next()
                self.ld([], [bw], wt[:], self.w("w_mod").ap()[l, :, cg * 512:(cg + 1) * 512].rearrange("(k p) n -> p k n", p=128))
                bank = cg % 2
                for k in range(8):
                    self.T([bw, bcs], [self.bpb[bank]], lambda: nc.tensor.matmul(
                        self.pb[bank][0:2, :], lhsT=cs[:, k:16:8], rhs=wt[:, k, :], start=(k == 0), stop=(k == 7)))
                self.V([self.bpb[bank], bbm], [bmodv], lambda: nc.vector.tensor_tensor(
                    out=modv[:, cg * 512:(cg + 1) * 512], in0=self.pb[bank][0:2, :], in1=bm[:, cg * 512:(cg + 1) * 512], op=ALU.add))
            self.ld([bmodv], [self.bMODV], self.MODV.ap(), modv[:])

    def mod_tables(self, es, l, gname, off_sh, off_sc, pfx, tmp_es=None):
        nc = self.nc
        gb = self.sbc(tmp_es or es, pfx + "gb", [128, D], F32); bg = Buf()
        self.ld([], [bg], gb[:], self.bc_ap(self.w(gname), l * D, D))
        Al, Sl = [], []
        bt = Buf()
        for r in range(2):
            a = self.sbc(es, f"{pfx}A{r}", [128, D], F32)
            s = self.sbc(es, f"{pfx}S{r}", [128, D], F32)
            self.ld([self.bMODV], [bt], a[:], self.bc_ap(self.MODV, r * 6 * D + off_sc, D))
            self.ld([self.bMODV], [bt], s[:], self.bc_ap(self.MODV, r * 6 * D + off_sh, D))
            self.V([bt, bg], [bt], lambda: nc.vector.scalar_tensor_tensor(
                out=a[:], in0=a[:], scalar=1.0, in1=gb[:], op0=ALU.add, op1=ALU.mult))
            Al.append(a); Sl.append(s)
        return Al, Sl, bt

    def norm_tile(self, xt, bx, xn, bxn, A, S, bt, scr, add_on_dve=False):
        nc = self.nc
        junk, bj, st, bst = scr
        self.V([], [bst], lambda: nc.vector.memset(st[:, 0:1], 0.0))
        self.A([bx], [bj, bst], lambda: nc.scalar.activation(out=junk[:], in_=xt, func=AF.Square, accum_out=st[:, 0:1]))
        self.V([bst], [bst], lambda: nc.vector.tensor_scalar(out=st[:, 1:2], in0=st[:, 0:1], scalar1=1.0 / D, scalar2=EPS, op0=ALU.mult, op1=ALU.add))
        self.A([bst], [bst], lambda: nc.scalar.activation(out=st[:, 2:3], in_=st[:, 1:2], func=AF.Sqrt))
        self.V([bst], [bst], lambda: nc.vector.reciprocal(out=st[:, 3:4], in_=st[:, 2:3]))
        self.V([bx, bst, bt], [bxn], lambda: nc.vector.scalar_tensor_tensor(
            out=xn, in0=xt, scalar=st[:, 3:4], in1=A[:], op0=ALU.mult, op1=ALU.mult))
        if add_on_dve:
            self.V([bxn, bt], [bxn], lambda: nc.vector.tensor_tensor(out=xn, in0=xn, in1=S[:], op=ALU.add))
        else:
            self.G([bxn, bt], [bxn], lambda: nc.gpsimd.tensor_tensor(out=xn, in0=xn, in1=S[:], op=ALU.add))

    def phase_norm_T(self, l):
        nc = self.nc
        with ExitStack() as es:
            Al, Sl, bt = self.mod_tables(es, l, "norm1_g", 0, D, "n1")
            xr = Rot(nc, es, "xt", [128, D], F32, 2)
            xnr = Rot(nc, es, "xn", [128, D], F32, 2)
            junk = self.sbc(es, "junk", [128, D], F32); bj = Buf()
            str_ = Rot(nc, es, "st", [128, 4], F32, 2)
            for t in range(NT):
                r = 0 if t < 32 else 1
                xt, bx = xr.next(); xn, bxn = xnr.next(); st, bst = str_.next()
                self.ld([self.bXS[t]], [bx], xt[:], self.XS.ap()[t * 128:(t + 1) * 128, :])
                self.norm_tile(xt[:], bx, xn[:], bxn, Al[r], Sl[r], bt, (junk, bj, st, bst))
                for half in range(2):
                    bank = (2 * t + half) % 4
                    for kk in range(4):
                        k = half * 4 + kk
                        self.T([bxn, self.b_const], [self.bpb[bank]], lambda: nc.tensor.transpose(
                            out=self.pb[bank][:, kk * 128:(kk + 1) * 128], in_=xn[:, k * 128:(k + 1) * 128], identity=self.ident32[:]))
                    cp = self.A if half == 0 else self.V
                    eng = nc.scalar.copy if half == 0 else nc.vector.tensor_copy
                    cp([self.bpb[bank]], [self.bh1T], lambda: eng(
                        out=self.h1T[:, half * 4:half * 4 + 4, t * 128:(t + 1) * 128],
                        in_=self.pb[bank][:, :].rearrange("p (a b) -> p a b", a=4)))

    def rope_fm(self, bank_a, bank_b, m, n, tok0, cosT, sinT, perm, btab, rs, out_ap, bout):
        nc = self.nc
        (asb_r, t1_r, t2_r) = rs
        asb, basb = asb_r.next(); t1, bt1 = t1_r.next(); t2, bt2 = t2_r.next()
        self.A([self.bpb[bank_a]], [basb], lambda: nc.scalar.copy(out=asb[0:m, 0:n], in_=self.pb[bank_a][0:m, 0:n]))
        self.T([basb, btab], [self.bpb[bank_b]], lambda: nc.tensor.matmul(
            self.pb[bank_b][0:m, 0:n], lhsT=perm[0:m, 0:m], rhs=asb[0:m, 0:n], start=True, stop=True))
        self.V([self.bpb[bank_a], btab], [bt1], lambda: nc.vector.tensor_tensor(
            out=t1[0:m, 0:n], in0=self.pb[bank_a][0:m, 0:n], in1=cosT[0:m, tok0:tok0 + n], op=ALU.mult))
        self.V([self.bpb[bank_b], btab], [bt2], lambda: nc.vector.tensor_tensor(
            out=t2[0:m, 0:n], in0=self.pb[bank_b][0:m, 0:n], in1=sinT[0:m, tok0:tok0 + n], op=ALU.mult))
        self.G([bt1, bt2], [bout], lambda: nc.gpsimd.tensor_tensor(out=out_ap, in0=t1[0:m, 0:n], in1=t2[0:m, 0:n], op=ALU.add))

    def tok_groups(self, with_ctx=True):
        g = [(i * 512, 512) for i in range(8)]
        if with_ctx:
            g.append((L, LC))
        return g

    def phase_ret(self, l, last):
        nc = self.nc
        with ExitStack() as es:
            cosT = self.sbc(es, "cosR", [128, L], F32); sinT = self.sbc(es, "sinR", [128, L], F32)
            perm = self.sbc(es, "permR", [128, 128], BF16)
            rtab = self.sbc(es, "rtab", [128, 6, 128], F32); rcol = self.sbc(es, "rcol", [128, 4], F32)
            btab = Buf()
            for t, nm in ((cosT, "cosR"), (sinT, "sinR"), (perm, "permR"), (rtab, "rtab"), (rcol, "rcol")):
                self.ld([], [btab], t[:], self.cin(nm).ap())
            lg = self.sbc(es, "lg", [128, 8], F32); blg = Buf()
            self.ld([], [blg], lg[:], self.bc_ap(self.w("ret_decay_logit"), l * 8, 8))
            self.A([blg], [blg], lambda: nc.scalar.activation(out=lg[:], in_=lg[:], func=AF.Exp, scale=-1.0))
            self.A([blg], [blg], lambda: nc.scalar.activation(out=lg[:], in_=lg[:], func=AF.Ln, bias=1.0))
            self.V([blg], [blg], lambda: nc.vector.tensor_scalar(out=lg[:], in0=lg[:], scalar1=-1.0, scalar2=None, op0=ALU.mult))
            qT = self.sbc(es, "qT", [128, NTOK], BF16); bq = Buf()
            kT = self.sbc(es, "kT", [128, NTOK], BF16); bk = Buf()
            sg = self.sbc(es, "sg", [128, NTOK], BF16); bsg = Buf()
            vtm = self.sbc(es, "vtm", [128, NT, 128], BF16); bv = Buf()
            kwf = self.sbc(es, "kwf", [128, NT, 128], BF16); bkwf = Buf()
            kwb = self.sbc(es, "kwb", [128, NT, 128], BF16); bkwb = Buf()
            sbp = self.sbc(es, "sbp", [128, NT, 128], BF16); bsbp = Buf()
            S32 = self.sbc(es, "S32", [128, 128], F32); bS = Buf()
            DT = self.sbc(es, "DT", [128, 128], F32); bDT = Buf()
            dtmp = self.sbc(es, "dtmp", [128, 128], F32)
            wrd = self.sbc(es, "wrd", [128, 2, 128], BF16); bwrd = Buf()
            wcol = self.sbc(es, "wcol", [128, 4], F32); bwcol = Buf()
            W4 = self.sbc(es, "W4", [128, 4, 8, 128], BF16); bW = Buf()
            rs = (Rot(nc, es, "asb", [128, 512], BF16, 2), Rot(nc, es, "t1", [128, 512], F32, 2), Rot(nc, es, "t2", [128, 512], F32, 2))
            sfr = Rot(nc, es, "sfb", [128, 128], BF16, 6)
            psb_r = Rot(nc, es, "psb", [128, 512], BF16, 2)
            qw_r = Rot(nc, es, "qw", [128, 2, 512], BF16, 2)
            ysq_r = Rot(nc, es, "ysq", [128, 512], BF16, 2)
            rst_r = Rot(nc, es, "rst", [128, 512], F32, 2)
            yt_r = Rot(nc, es, "yt", [128, 512], F32, 2)
            ro_r = Rot(nc, es, "ro", [128, 512], BF16, 2)
            ks = 128.0 ** -0.5
            for h in range(4):
                lgf = lg[:, h:h + 1]; lgb = lg[:, 4 + h:5 + h]
                self.A([blg, btab], [bDT], lambda: nc.scalar.activation(out=DT[:], in_=rtab[:, 0, :], func=AF.Exp, scale=lgf))
                self.V([bDT, btab], [bDT], lambda: nc.vector.tensor_tensor(out=DT[:], in0=DT[:], in1=rtab[:, 1, :], op=ALU.mult))
                self.A([blg, btab], [bDT], lambda: nc.scalar.activation(out=dtmp[:], in_=rtab[:, 2, :], func=AF.Exp, scale=lgb))
                self.V([bDT, btab], [bDT], lambda: nc.vector.tensor_tensor(out=dtmp[:], in0=dtmp[:], in1=rtab[:, 3, :], op=ALU.mult))
                self.V([bDT], [bDT], lambda: nc.vector.tensor_tensor(out=DT[:], in0=DT[:], in1=dtmp[:], op=ALU.add))
                self.A([blg, btab], [bwrd], lambda: nc.scalar.activation(out=wrd[:, 0, :], in_=rtab[:, 4, :], func=AF.Exp, scale=lgf))
                self.A([blg, btab], [bwrd], lambda: nc.scalar.activation(out=wrd[:, 1, :], in_=rtab[:, 5, :], func=AF.Exp, scale=lgb))
                self.A([blg, btab], [bwcol], lambda: nc.scalar.activation(out=wcol[:, 0:1], in_=rcol[:, 0:1], func=AF.Exp, scale=lgf))
                self.A([blg, btab], [bwcol], lambda: nc.scalar.activation(out=wcol[:, 1:2], in_=rcol[:, 1:2], func=AF.Exp, scale=lgb))
                self.A([blg, btab], [bwcol], lambda: nc.scalar.activation(out=wcol[:, 2:3], in_=rcol[:, 2:3], func=AF.Exp, scale=lgf))
                self.A([blg, btab], [bwcol], lambda: nc.scalar.activation(out=wcol[:, 3:4], in_=rcol[:, 2:3], func=AF.Exp, scale=lgb))
                self.V([bwcol], [bwcol], lambda: nc.vector.tensor_scalar(out=wcol[:, 0:2], in0=wcol[:, 0:2], scalar1=ks, scalar2=None, op0=ALU.mult))
                for i, c0 in enumerate((C_RQ, C_RK, C_RV, C_RG)):
                    self.ldcast([], [bW], W4[:, i, :, :], self.wslice("w_in", l, c0 + h * 128, 128))
                for (tok0, n) in self.tok_groups():
                    lat = tok0 < L
                    for i, (dst, bdst) in enumerate(((qT, bq), (kT, bk))):
                        bank = i
                        self.proj(bank, W4[:, i], bW, 0, 128, tok0, n)
                        if lat:
                            self.rope_fm(bank, 2 + i, 128, n, tok0, cosT, sinT, perm, btab, rs, dst[:, tok0:tok0 + n], bdst)
                        else:
                            self.A([self.bpb[bank]], [bdst], lambda: nc.scalar.copy(out=dst[:, tok0:tok0 + n], in_=self.pb[bank][:, 0:n]))
                    self.proj(4, W4[:, 3], bW, 0, 128, tok0, n)
                    self.A([self.bpb[4]], [bsg], lambda: nc.scalar.activation(out=sg[:, tok0:tok0 + n], in_=self.pb[4][:, 0:n], func=AF.Silu))
                    nt = n // 128
                    for j in range(nt):
                        for k in range(8):
                            self.T([bW, self.bh1T], [self.bpb[5]], lambda: nc.tensor.matmul(
                                self.pb[5][:, j * 128:(j + 1) * 128], lhsT=self.h1T[:, k, tok0 + j * 128:tok0 + (j + 1) * 128],
                                rhs=W4[:, 2, k, :], start=(k == 0), stop=(k == 7)))
                    self.V([self.bpb[5]], [bv], lambda: nc.vector.tensor_copy(
                        out=vtm[:, tok0 // 128:tok0 // 128 + nt, :], in_=self.pb[5][:, 0:n].rearrange("p (a b) -> p a b", b=128)))
                for t0 in range(0, NT, 4):
                    nt = min(4, NT - t0)
                    bank = 6 + (t0 // 4) % 2
                    pbb = self.pb[bank][:, :].bitcast(BF16)
                    for j in range(nt):
                        self.T([bk, self.b_const], [self.bpb[bank]], lambda: nc.tensor.transpose(
                            out=pbb[:, j * 128:(j + 1) * 128], in_=kT[:, (t0 + j) * 128:(t0 + j + 1) * 128], identity=self.identb[:]))
                    src = pbb[:, 0:nt * 128].rearrange("p (a b) -> p a b", b=128)
                    self.V([self.bpb[bank], bwcol], [bkwf], lambda: nc.vector.tensor_scalar(
                        out=kwf[:, t0:t0 + nt, :], in0=src, scalar1=wcol[:, 0:1], scalar2=None, op0=ALU.mult))
                    self.A([self.bpb[bank], bwcol], [bkwb], lambda: nc.scalar.activation(
                        out=kwb[:, t0:t0 + nt, :], in_=src, func=AF.Identity, scale=wcol[:, 1:2]))
                self.V([], [bS], lambda: nc.vector.memset(S32[:], 0.0))
                for idx, n_ in enumerate([33, 32] + list(range(31, -1, -1))):
                    bank = 6 + idx % 2
                    self.V([bS], [bsbp], lambda: nc.vector.tensor_copy(out=sbp[:, n_, :], in_=S32[:]))
                    self.T([bkwb, bv], [self.bpb[bank]], lambda: nc.tensor.matmul(
                        self.pb[bank][:, 0:128], lhsT=kwb[:, n_, :], rhs=vtm[:, n_, :], start=True, stop=True))
                    self.V([self.bpb[bank], bS, bwcol], [bS], lambda: nc.vector.scalar_tensor_tensor(
                        out=S32[:], in0=S32[:], scalar=wcol[:, 3:4], in1=self.pb[bank][:, 0:128], op0=ALU.mult, op1=ALU.add))
                self.V([], [bS], lambda: nc.vector.memset(S32[:], 0.0))
                groups = [(32, 2)] + [(c0, 4) for c0 in range(0, 32, 4)]
                for gi, (c0, ncnk) in enumerate(groups):
                    isctx = c0 >= 32
                    want_out = (not isctx) or (not last)
                    tok0 = c0 * 128; n = ncnk * 128
                    sfs = []
                    for c in range(ncnk):
                        n_ = c0 + c
                        sf, bsf = sfr.next(); sfs.append((sf, bsf))
                        self.V([bS], [bsf], lambda: nc.vector.tensor_copy(out=sf[:], in_=S32[:]))
                        bank = 6 + c % 2
                        self.T([bkwf, bv], [self.bpb[bank]], lambda: nc.tensor.matmul(
                            self.pb[bank][:, 0:128], lhsT=kwf[:, n_, :], rhs=vtm[:, n_, :], start=True, stop=True))
                        self.V([self.bpb[bank], bS, bwcol], [bS], lambda: nc.vector.scalar_tensor_tensor(
                            out=S32[:], in0=S32[:], scalar=wcol[:, 2:3], in1=self.pb[bank][:, 0:128], op0=ALU.mult, op1=ALU.add))
                    if not want_out:
                        continue
                    bs_, bo_ = (0, 1) if gi % 2 == 0 else (2, 3)
                    for c in range(ncnk):
                        self.T([bk, bq], [self.bpb[bs_]], lambda: nc.tensor.matmul(
                            self.pb[bs_][:, c * 128:(c + 1) * 128], lhsT=kT[:, tok0 + c * 128:tok0 + (c + 1) * 128],
                            rhs=qT[:, tok0 + c * 128:tok0 + (c + 1) * 128], start=True, stop=True))
                    psb, bpsb = psb_r.next()
                    self.V([self.bpb[bs_], bDT], [bpsb], lambda: nc.vector.tensor_tensor(
                        out=psb[:, 0:n].rearrange("p (a b) -> p a b", b=128), in0=self.pb[bs_][:, 0:n].rearrange("p (a b) -> p a b", b=128),
                        in1=bass.AP(tensor=DT[:].tensor, offset=DT[:].offset, ap=[list(DT[:].ap[0]), [0, ncnk], [1, 128]]), op=ALU.mult))
                    qw, bqw = qw_r.next()
                    for d_ in range(2):
                        wr_ap = wrd[:, d_, :]
                        self.G([bq, bwrd], [bqw], lambda: nc.gpsimd.tensor_tensor(
                            out=qw[:, d_, 0:n].rearrange("p (a b) -> p a b", b=128), in0=qT[:, tok0:tok0 + n].rearrange("p (a b) -> p a b", b=128),
                            in1=bass.AP(tensor=wr_ap.tensor, offset=wr_ap.offset, ap=[list(wr_ap.ap[0]), [0, ncnk], [1, 128]]), op=ALU.mult))
                    for c in range(ncnk):
                        n_ = c0 + c
                        o = self.pb[bo_][:, c * 128:(c + 1) * 128]
                        self.T([bv, bpsb], [self.bpb[bo_]], lambda: nc.tensor.matmul(o, lhsT=vtm[:, n_, :], rhs=psb[:, c * 128:(c + 1) * 128], start=True, stop=False))
                        self.T([sfs[c][1], bqw], [self.bpb[bo_]], lambda: nc.tensor.matmul(o, lhsT=sfs[c][0][:], rhs=qw[:, 0, c * 128:(c + 1) * 128], start=False, stop=False))
                        self.T([bsbp, bqw], [self.bpb[bo_]], lambda: nc.tensor.matmul(o, lhsT=sbp[:, n_, :], rhs=qw[:, 1, c * 128:(c + 1) * 128], start=False, stop=True))
                    ysq, bysq = ysq_r.next()
                    self.A([self.bpb[bo_]], [bysq], lambda: nc.scalar.activation(out=ysq[:, 0:n], in_=self.pb[bo_][:, 0:n], func=AF.Square))
                    bm_ = 4 + gi % 2
                    self.T([bysq, self.b_const], [self.bpb[bm_]], lambda: nc.tensor.matmul(
                        self.pb[bm_][:, 0:n], lhsT=self.onesb[:], rhs=ysq[:, 0:n], start=True, stop=True))
                    rst, brst = rst_r.next()
                    self.V([self.bpb[bm_]], [brst], lambda: nc.vector.tensor_scalar(out=rst[:, 0:n], in0=self.pb[bm_][:, 0:n], scalar1=EPS, scalar2=None, op0=ALU.add))
                    self.A([brst], [brst], lambda: nc.scalar.activation(out=rst[:, 0:n], in_=rst[:, 0:n], func=AF.Sqrt))
                    self.V([brst], [brst], lambda: nc.vector.reciprocal(out=rst[:, 0:n], in_=rst[:, 0:n]))
                    yt, byt = yt_r.next()
                    self.V([self.bpb[bo_], brst], [byt], lambda: nc.vector.tensor_tensor(out=yt[:, 0:n], in0=self.pb[bo_][:, 0:n], in1=rst[:, 0:n], op=ALU.mult))
                    ro, bro = ro_r.next()
                    self.G([byt, bsg], [bro], lambda: nc.gpsimd.tensor_tensor(out=ro[:, 0:n], in0=yt[:, 0:n], in1=sg[:, tok0:tok0 + n], op=ALU.mult))
                    self.ld([bro], [self.bMIX[0][h]], self.MIX.ap()[0, h * 128:(h + 1) * 128, tok0:tok0 + n], ro[:, 0:n])

    def phase_att(self, l, last):
        nc = self.nc
        with ExitStack() as es:
            cosT = self.sbc(es, "cosA", [64, L], F32); sinT = self.sbc(es, "sinA", [64, L], F32)
            perm = self.sbc(es, "permA", [64, 64], BF16)
            amask = self.sbc(es, "amask", [128, 2, 128], BF16)
            btab = Buf()
            for t, nm in ((cosT, "cosA"), (sinT, "sinA"), (perm, "permA"), (amask, "amask")):
                self.ld([], [btab], t[:], self.cin(nm).ap())
            esink = self.sbc(es, "esink", [128, 8], F32); bes = Buf()
            self.ld([], [bes], esink[:], self.bc_ap(self.w("attn_sink"), l * 8, 8))
            self.A([bes], [bes], lambda: nc.scalar.activation(out=esink[:], in_=esink[:], func=AF.Exp))
            Wq = self.sbc(es, "Wq", [128, 8, 256], BF16); Wk = self.sbc(es, "Wk", [128, 8, 64], BF16); Wv = self.sbc(es, "Wv", [128, 8, 64], BF16)
            bW = Buf()
            kT = self.sbc(es, "akT", [64, NTOK], BF16); bk = Buf()
            qT = self.sbc(es, "aqT", [64, NT, 4, 128], BF16); bq = Buf()
            v1 = self.sbc(es, "av1", [128, NT, 65], BF16); bv = Buf()
            rs = (Rot(nc, es, "asb", [64, 512], BF16, 2), Rot(nc, es, "t1", [64, 512], F32, 2), Rot(nc, es, "t2", [64, 512], F32, 2))
            e_r = Rot(nc, es, "E", [128, 512], BF16, 10)
            den_r = Rot(nc, es, "den", [128, 8], F32, 2)
            atm_r = Rot(nc, es, "atm", [128, 256], F32, 2)
            ao_r = Rot(nc, es, "ao", [128, 2, 512], BF16, 2)
            self.V([], [bv], lambda: nc.vector.memset(v1[:], 1.0))
            for g in range(2):
                self.ldcast([], [bW], Wq[:], self.wslice("w_in", l, C_AQ + g * 256, 256))
                self.ldcast([], [bW], Wk[:], self.wslice("w_in", l, C_AK + g * 64, 64))
                self.ldcast([], [bW], Wv[:], self.wslice("w_in", l, C_AV + g * 64, 64))
                for (tok0, n) in self.tok_groups():
                    lat = tok0 < L
                    nt = n // 128
                    self.proj(0, Wk, bW, 0, 64, tok0, n)
                    if lat:
                        self.rope_fm(0, 2, 64, n, tok0, cosT, sinT, perm, btab, rs, kT[:, tok0:tok0 + n], bk)
                    else:
                        self.A([self.bpb[0]], [bk], lambda: nc.scalar.copy(out=kT[:, tok0:tok0 + n], in_=self.pb[0][0:64, 0:n]))
                    for hh in range(4):
                        bank = hh % 2
                        self.proj(bank, Wq, bW, hh * 64, 64, tok0, n)
                        dst = qT[:, tok0 // 128:tok0 // 128 + nt, hh, :]
                        if lat:
                            self._rope_q(bank, 2 + hh % 2, n, tok0, cosT, sinT, perm, btab, rs, dst, bq)
                        else:
                            self.A([self.bpb[bank]], [bq], lambda: nc.scalar.copy(out=dst, in_=self.pb[bank][0:64, 0:n].rearrange("p (a b) -> p a b", b=128)))
                    for j in range(nt):
                        for k in range(8):
                            self.T([bW, self.bh1T], [self.bpb[4]], lambda: nc.tensor.matmul(
                                self.pb[4][:, j * 64:(j + 1) * 64], lhsT=self.h1T[:, k, tok0 + j * 128:tok0 + (j + 1) * 128],
                                rhs=Wv[:, k, :], start=(k == 0), stop=(k == 7)))
                    self.V([self.bpb[4]], [bv], lambda: nc.vector.tensor_copy(
                        out=v1[:, tok0 // 128:tok0 // 128 + nt, 0:64], in_=self.pb[4][:, 0:nt * 64].rearrange("p (a b) -> p a b", b=64)))
                qtiles = list(range(32)) + ([] if last else [32, 33])
                for qi, qt in enumerate(qtiles):
                    if qt < 32:
                        keys = ([(qt - 1, 0)] if qt > 0 else []) + [(qt, None)] + ([(qt + 1, 1)] if qt < 31 else []) + [(32, None), (33, None)]
                    else:
                        keys = [(32, None), (33, None)]
                    bo_ = 5 + qi % 2
                    Es = []
                    for idx, (kt, mk) in enumerate(keys):
                        bs_ = idx
                        self.T([bk, bq], [self.bpb[bs_]], lambda: nc.tensor.matmul(
                            self.pb[bs_][:, :], lhsT=kT[:, kt * 128:(kt + 1) * 128], rhs=qT[:, qt, :, :].rearrange("p a b -> p (a b)"), start=True, stop=True))
                        E, bE = e_r.next()
                        self.A([self.bpb[bs_]], [bE], lambda: nc.scalar.activation(out=E[:], in_=self.pb[bs_][:, :], func=AF.Exp, scale=0.125))
                        if mk is not None:
                            m_ap = amask[:, mk, :]
                            self.G([bE, btab], [bE], lambda: nc.gpsimd.tensor_tensor(
                                out=E[:].rearrange("p (a b) -> p a b", b=128), in0=E[:].rearrange("p (a b) -> p a b", b=128),
                                in1=bass.AP(tensor=m_ap.tensor, offset=m_ap.offset, ap=[list(m_ap.ap[0]), [0, 4], [1, 128]]), op=ALU.mult))
                        Es.append((E, bE))
                    for hh in range(4):
                        for idx, (kt, mk) in enumerate(keys):
                            E, bE = Es[idx]
                            self.T([bE, bv], [self.bpb[bo_]], lambda: nc.tensor.matmul(
                                self.pb[bo_][:, hh * 65:(hh + 1) * 65], lhsT=E[:, hh * 128:(hh + 1) * 128], rhs=v1[:, kt, :],
                                start=(idx == 0), stop=(idx == len(keys) - 1)))
                    den, bden = den_r.next()
                    o3 = self.pb[bo_][:, 0:260].rearrange("p (a b) -> p a b", b=65)
                    self.V([self.bpb[bo_], bes], [bden], lambda: nc.vector.tensor_tensor(
                        out=den[:, 0:4], in0=self.pb[bo_][:, 64:260:65], in1=esink[:, g * 4:(g + 1) * 4], op=ALU.add))
                    self.V([bden], [bden], lambda: nc.vector.reciprocal(out=den[:, 4:8], in_=den[:, 0:4]))
                    atm, batm = atm_r.next()
                    self.V([self.bpb[bo_], bden], [batm], lambda: nc.vector.tensor_tensor(
                        out=atm[:].rearrange("p (a b) -> p a b", b=64), in0=o3[:, :, 0:64], in1=bass.AP(tensor=den[:].tensor, offset=den[:, 4:8].offset, ap=[list(den[:].ap[0]), [1, 4], [0, 64]]), op=ALU.mult))
                    j = qi % 4
                    if j == 0:
                        ao, bao = ao_r.next()
                        qt0 = qt
                    for c in range(2):
                        self.T([batm, self.b_const], [self.bpb[7]], lambda: nc.tensor.transpose(
                            out=self.pb[7][:, c * 256 + (j % 2) * 128:c * 256 + (j % 2) * 128 + 128], in_=atm[:, c * 128:(c + 1) * 128], identity=self.ident32[:]))
                    if j % 2 == 1 or qi == len(qtiles) - 1:
                        nn = (j % 2 + 1) * 128
                        off = (j // 2) * 256
                        for c in range(2):
                            cp = self.A if c == 0 else self.V
                            eng = nc.scalar.copy if c == 0 else nc.vector.tensor_copy
                            cp([self.bpb[7]], [bao], lambda: eng(out=ao[:, c, off:off + nn], in_=self.pb[7][:, c * 256:c * 256 + nn]))
                    if j == 3 or qi == len(qtiles) - 1:
                        nn = (j + 1) * 128
                        for c in range(2):
                            self.ld([bao], [self.bMIX[1][2 * g + c]], self.MIX.ap()[1, (2 * g + c) * 128:(2 * g + c + 1) * 128, qt0 * 128:qt0 * 128 + nn], ao[:, c, 0:nn])

    def _rope_q(self, bank_a, bank_b, n, tok0, cosT, sinT, perm, btab, rs, dst, bq):
        nc = self.nc
        asb, basb = rs[0].next(); t1, bt1 = rs[1].next(); t2, bt2 = rs[2].next()
        self.A([self.bpb[bank_a]], [basb], lambda: nc.scalar.copy(out=asb[0:64, 0:n], in_=self.pb[bank_a][0:64, 0:n]))
        self.T([basb, btab], [self.bpb[bank_b]], lambda: nc.tensor.matmul(
            self.pb[bank_b][0:64, 0:n], lhsT=perm[0:64, 0:64], rhs=asb[0:64, 0:n], start=True, stop=True))
        self.V([self.bpb[bank_a], btab], [bt1], lambda: nc.vector.tensor_tensor(
            out=t1[0:64, 0:n], in0=self.pb[bank_a][0:64, 0:n], in1=cosT[0:64, tok0:tok0 + n], op=ALU.mult))
        self.V([self.bpb[bank_b], btab], [bt2], lambda: nc.vector.tensor_tensor(
            out=t2[0:64, 0:n], in0=self.pb[bank_b][0:64, 0:n], in1=sinT[0:64, tok0:tok0 + n], op=ALU.mult))
        self.G([bt1, bt2], [bq], lambda: nc.gpsimd.tensor_tensor(
            out=dst, in0=t1[0:64, 0:n].rearrange("p (a b) -> p a b", b=128), in1=t2[0:64, 0:n].rearrange("p (a b) -> p a b", b=128), op=ALU.add))

    def hy_filter(self, es, l, Lq, embname, tlname, width, Gd, bG, skipc, bskip, fcols, bfc, w1, w2, w3, bw):
        nc = self.nc
        ncol = 2 * Lq - 1
        ngrp = width // 512
        with ExitStack() as fs:
            nd = self.sbc(fs, "nd", [128, 4], F32)
            be = Buf()
            self.ld([], [be], nd[:], self.cin("ndelta").ap())
            h2 = self.sbc(fs, "h2", [64, width], BF16); bh2 = Buf()
            emb_r = Rot(nc, fs, "emb", [33, 512], F32, 2)
            tl_r = Rot(nc, fs, "tl", [128, 512], F32, 2)
            a_r = Rot(nc, fs, "farg", [64, 512], F32, 2)
            h1_r = Rot(nc, fs, "fh1", [64, 512], F32, 2)
            rr = (Rot(nc, fs, "rrt", [64, 512], F32, 2), Rot(nc, fs, "rri", [64, 512], mybir.dt.int32, 2))
            for gidx in range(ngrp):
                cs = slice(gidx * 512, (gidx + 1) * 512)
                emb, bemb = emb_r.next()
                self.ld([], [bemb], emb[:], self.cin(embname).ap()[:, cs])
                self.T([bemb, bw], [self.bpb[0]], lambda: nc.tensor.matmul(self.pb[0][0:64, :], lhsT=w1[:, :], rhs=emb[:, :], start=True, stop=True))
                a, ba = a_r.next()
                self.V([self.bpb[0], bfc], [ba], lambda: nc.vector.tensor_scalar(
                    out=a[:], in0=self.pb[0][0:64, :], scalar1=fcols[0:64, 0, 0:1], scalar2=fcols[0:64, 0, 1:2], op0=ALU.mult, op1=ALU.add))
                self.range_reduce(a, ba, rr)
                h1, bh1 = h1_r.next()
                self.A([ba], [bh1], lambda: nc.scalar.activation(out=h1[:], in_=a[:], func=AF.Sin))
                self.T([bh1, bw], [self.bpb[1]], lambda: nc.tensor.matmul(self.pb[1][0:64, :], lhsT=w2[:, :], rhs=h1[:], start=True, stop=True))
                a, ba = a_r.next()
                self.V([self.bpb[1], bfc], [ba], lambda: nc.vector.tensor_scalar(
                    out=a[:], in0=self.pb[1][0:64, :], scalar1=fcols[0:64, 0, 2:3], scalar2=fcols[0:64, 0, 3:4], op0=ALU.mult, op1=ALU.add))
                self.range_reduce(a, ba, rr)
                self.A([ba], [bh2], lambda: nc.scalar.activation(out=h2[:, cs], in_=a[:], func=AF.Sin))
            segs = []
            c0 = 0
            while c0 < ncol:
                lim = Lq if c0 < Lq else ncol
                n = min(512, lim - c0)
                segs.append((c0, n, c0 < Lq))
                c0 += n
            assert len(segs) <= 16
            graw = self.sbc(fs, "graw", [128, width], F32); bgr = Buf()
            dec_r = Rot(nc, fs, "dec", [128, 512], F32, 2)
            l1p = self.sbc(fs, "l1p", [128, 20], F32); bl1 = Buf()
            junk = self.sbc(fs, "fjunk", [128, 512], F32); bj = Buf()
            gb = self.sbc(fs, "gbf", [128, width], BF16); bgb = Buf()
            for i in range(4):
                self.V([], [bl1], lambda: nc.vector.memset(l1p[:], 0.0))
                for si, (c0, n, fwd) in enumerate(segs):
                    wc = (0 if fwd else 512) + i * 128
                    bank = 2 + si % 2
                    self.T([bh2, bw], [self.bpb[bank]], lambda: nc.tensor.matmul(
                        self.pb[bank][:, 0:n], lhsT=w3[:, wc:wc + 128], rhs=h2[:, c0:c0 + n], start=True, stop=True))
                    tl, btl = tl_r.next()
                    self.ld([], [btl], tl[:, 0:n], self.bc_ap(self.cin(tlname), c0, n))
                    dec, bdec = dec_r.next()
                    self.A([btl, be], [bdec], lambda: nc.scalar.activation(out=dec[:, 0:n], in_=tl[:, 0:n], func=AF.Exp, scale=nd[:, i:i + 1]))
                    self.V([self.bpb[bank], bdec], [bgr], lambda: nc.vector.tensor_tensor(
                        out=graw[:, c0:c0 + n], in0=self.pb[bank][:, 0:n], in1=dec[:, 0:n], op=ALU.mult))
                    self.A([bgr], [bj, bl1], lambda: nc.scalar.activation(out=junk[:, 0:n], in_=graw[:, c0:c0 + n], func=AF.Abs, accum_out=l1p[:, si:si + 1]))
                self.V([bl1], [bl1], lambda: nc.vector.reduce_sum(out=l1p[:, 16:17], in_=l1p[:, 0:16], axis=mybir.AxisListType.X))
                self.V([bl1], [bl1], lambda: nc.vector.reciprocal(out=l1p[:, 17:18], in_=l1p[:, 16:17]))
                self.V([bgr, bl1], [bgr], lambda: nc.vector.tensor_scalar(out=graw[:, 0:ncol], in0=graw[:, 0:ncol], scalar1=l1p[:, 17:18], scalar2=None, op0=ALU.mult))
                self.V([bgr, bskip], [bgr], lambda: nc.vector.tensor_tensor(out=graw[:, Lq - 1:Lq], in0=graw[:, Lq - 1:Lq], in1=skipc[:, i, 0:1], op=ALU.add))
                self.A([bgr], [bgb], lambda: nc.scalar.copy(out=gb[:, 0:ncol], in_=graw[:, 0:ncol]))
                self.ld([bgb], [bG[i]], Gd.ap()[i * 128:(i + 1) * 128, 0:ncol], gb[:, 0:ncol])

    def range_reduce(self, a, ba, rr):
        nc = self.nc
        TWO_PI = 2.0 * math.pi
        t, bt_ = rr[0].next(); ti, bti = rr[1].next()
        self.V([ba], [bt_], lambda: nc.vector.tensor_scalar(out=t[:], in0=a[:], scalar1=1.0 / TWO_PI, scalar2=None, op0=ALU.mult))
        self.V([bt_], [bti], lambda: nc.vector.tensor_copy(out=ti[:], in_=t[:]))
        self.V([bti], [bt_], lambda: nc.vector.tensor_copy(out=t[:], in_=ti[:]))
        self.V([bt_, ba], [ba], lambda: nc.vector.scalar_tensor_tensor(out=a[:], in0=t[:], scalar=-TWO_PI, in1=a[:], op0=ALU.mult, op1=ALU.add))
        self.V([ba], [bt_], lambda: nc.vector.tensor_scalar(out=t[:], in0=a[:], scalar1=0.0, scalar2=TWO_PI, op0=ALU.is_lt, op1=ALU.mult))
        self.V([bt_, ba], [ba], lambda: nc.vector.tensor_tensor(out=a[:], in0=a[:], in1=t[:], op=ALU.add))
        self.V([ba], [ba], lambda: nc.vector.tensor_scalar(out=a[:], in0=a[:], scalar1=-math.pi, scalar2=None, op0=ALU.add))

    def phase_hy(self, l, last):
        nc = self.nc
        with ExitStack() as es:
            skipc, bskip = self.load_cols(es, "skipc", self.w("hy_skip").ap()[l], 1, 512)
            cwc, bcw = self.load_cols(es, "cwc", self.w("hy_conv_w").ap()[l], 3, 1536)
            cbc, bcb = self.load_cols(es, "cbc", self.w("hy_conv_b").ap()[l], 1, 1536)
            frow = self.sbc(es, "frow", [4, 64], F32); bfr = Buf()
            for i, nm in enumerate(("hy_freq1", "hy_b1", "hy_freq2", "hy_b2")):
                self.ld([], [bfr], frow[i:i + 1, :], self.w(nm).ap()[l])
            fcols = self.sbc(es, "fcols", [64, 1, 4], F32); bfc = Buf()
            self.T([bfr, self.b_const], [self.bpb[7]], lambda: nc.tensor.transpose(out=self.pb[7][0:64, 0:4], in_=frow[:, :], identity=self.ident32[0:4, 0:4]))
            self.V([self.bpb[7]], [bfc], lambda: nc.vector.tensor_copy(out=fcols[:, 0, :], in_=self.pb[7][0:64, 0:4]))
            self.V([bfc], [bfc], lambda: nc.vector.tensor_tensor(out=fcols[:, 0, 1:2], in0=fcols[:, 0, 1:2], in1=fcols[:, 0, 0:1], op=ALU.mult))
            self.V([bfc], [bfc], lambda: nc.vector.tensor_tensor(out=fcols[:, 0, 3:4], in0=fcols[:, 0, 3:4], in1=fcols[:, 0, 2:3], op=ALU.mult))
            self.V([bfc], [bfc], lambda: nc.vector.tensor_scalar(out=fcols[:, 0, 1:2], in0=fcols[:, 0, 1:2], scalar1=17.0 * math.pi, scalar2=None, op0=ALU.add))
            self.V([bfc], [bfc], lambda: nc.vector.tensor_scalar(out=fcols[:, 0, 3:4], in0=fcols[:, 0, 3:4], scalar1=17.0 * math.pi, scalar2=None, op0=ALU.add))
            w1 = self.sbc(es, "fw1", [33, 64], F32); w2 = self.sbc(es, "fw2", [64, 64], F32); w3 = self.sbc(es, "fw3", [64, 1024], BF16)
            bw = Buf()
            self.ld([], [bw], w1[:], self.w("hy_w1").ap()[l]); self.ld([], [bw], w2[:], self.w("hy_w2").ap()[l]); self.ldcast([], [bw], w3[:], self.w("hy_w3").ap()[l])
            self.hy_filter(es, l, L, "embL", "tlinL", 8192, self.GS, self.bGS, skipc, bskip, fcols, bfc, w1, w2, w3, bw)
            self.fw.barrier()
            if not last:
                self.hy_filter(es, l, LC, "embC", "tlinC", 512, self.GC, self.bGC, skipc, bskip, fcols, bfc, w1, w2, w3, bw)
                self.fw.barrier()
            Yc = self.sbc(es, "hYc", [128, 3, NTOK], BF16); bYc = Buf()
            zT = self.sbc(es, "hzT", [128, NTOK], BF16); bz = Buf()
            Z = self.sbc(es, "hZ", [128, 128, NT], BF16); bZ = Buf()
            ysb = self.sbc(es, "hysb", [128, NT, 128], BF16); bys = Buf()
            ho_r = Rot(nc, es, "ho", [128, 512], BF16, 2)
            LOFF, COFF = 1, L + 3
            segs = [(LOFF, 0, L)] + ([] if last else [(COFF, L, LC)])
            ntok = NTOK if not last else L
            ntile = ntok // 128
            for i in range(4):
                with ExitStack() as sa:
                    W3 = self.sbc(sa, "hW3", [128, 3, 8, 128], BF16); bW = Buf()
                    U = self.sbc(sa, "hU", [128, NTOK + 4], F32); bU = Buf()
                    yt_r = Rot(nc, sa, "hyt", [128, 512], F32, 2)
                    self.V([], [bU], lambda: nc.vector.memset(U[:], 0.0))
                    for s_ in range(3):
                        self.ldcast([], [bW], W3[:, s_, :, :], self.wslice("w_in", l, C_HU + s_ * 512 + i * 128, 128))
                    for s_ in range(3):
                        ch = s_ * 4 + i
                        for gi_, (tok0, n) in enumerate(self.tok_groups(not last)):
                            uoff = (LOFF if tok0 < L else COFF - L) + tok0
                            bank = gi_ % 2
                            self.proj(bank, W3[:, s_], bW, 0, 128, tok0, n)
                            cp = self.A if gi_ % 2 == 0 else self.V
                            eng = nc.scalar.copy if gi_ % 2 == 0 else nc.vector.tensor_copy
                            cp([self.bpb[bank]], [bU], lambda: eng(out=U[:, uoff:uoff + n], in_=self.pb[bank][:, 0:n]))
                        for (uo, t0, nseq) in segs:
                            for p0 in range(0, nseq, 512):
                                n = min(512, nseq - p0)
                                yt, byt = yt_r.next()
                                self.A([bU, bcw, bcb], [byt], lambda: nc.scalar.activation(
                                    out=yt[:, 0:n], in_=U[:, uo + p0:uo + p0 + n], func=AF.Identity, scale=cwc[:, ch, 1:2], bias=cbc[:, ch, 0:1]))
                                self.V([bU, byt, bcw], [byt], lambda: nc.vector.scalar_tensor_tensor(
                                    out=yt[:, 0:n], in0=U[:, uo + p0 - 1:uo + p0 - 1 + n], scalar=cwc[:, ch, 0:1], in1=yt[:, 0:n], op0=ALU.mult, op1=ALU.add))
                                self.V([bU, byt, bcw], [bYc], lambda: nc.vector.scalar_tensor_tensor(
                                    out=Yc[:, s_, t0 + p0:t0 + p0 + n], in0=U[:, uo + p0 + 1:uo + p0 + 1 + n], scalar=cwc[:, ch, 2:3], in1=yt[:, 0:n], op0=ALU.mult, op1=ALU.add))
                    self.G([bYc], [bz], lambda: nc.gpsimd.tensor_tensor(out=zT[:, 0:ntok], in0=Yc[:, 1, 0:ntok], in1=Yc[:, 2, 0:ntok], op=ALU.mult))
                    for t0 in range(0, ntile, 4):
                        nt = min(4, ntile - t0)
                        bank = 6 + (t0 // 4) % 2
                        pbb = self.pb[bank][:, :].bitcast(BF16)
                        for j in range(nt):
                            self.T([bz, self.b_const], [self.bpb[bank]], lambda: nc.tensor.transpose(
                                out=pbb[:, j * 128:(j + 1) * 128], in_=zT[:, (t0 + j) * 128:(t0 + j + 1) * 128], identity=self.identb[:]))
                        self.V([self.bpb[bank]], [bZ], lambda: nc.vector.tensor_copy(
                            out=Z[:, :, t0:t0 + nt].rearrange("p c t -> p t c"), in_=pbb[:, 0:nt * 128].rearrange("p (a b) -> p a b", b=128)))
                    self.fw.barrier()
                with ExitStack() as sb_:
                    tr = Rot(nc, sb_, "hT", [128, 8064], BF16, 3)
                    tcx = self.sbc(sb_, "hTc", [128, 16, 384], BF16); btc = Buf()
                    order = [31] + [j for j in range(63) if j != 31]
                    for c in range(128):
                        Tt, bT = tr.next()
                        c_abs = i * 128 + c
                        self.ld([self.bGS[i]], [bT], Tt[:], bass.AP(tensor=self.GS, offset=c_abs * 8192, ap=[[1, 128], [1, 8064]]))
                        bank = (c // 16) % 2
                        col0 = (c % 16) * 32
                        for idx, j in enumerate(order):
                            d = 31 - j
                            s_lo, s_hi = (0, 32 - d) if d >= 0 else (-d, 32)
                            self.T([bT, bZ], [self.bpb[bank]], lambda: nc.tensor.matmul(
                                self.pb[bank][:, col0 + s_lo + d:col0 + s_hi + d], lhsT=Tt[:, j * 128:(j + 1) * 128], rhs=Z[:, c, s_lo:s_hi],
                                start=(idx == 0), stop=(idx == 62)))
                        if c % 16 == 15:
                            cb = c - 15
                            cp = self.A if (c // 16) % 2 == 0 else self.V
                            eng = nc.scalar.copy if (c // 16) % 2 == 0 else nc.vector.tensor_copy
                            cp([self.bpb[bank]], [bys], lambda: eng(
                                out=ysb[:, 0:32, cb:cb + 16].rearrange("p t c -> p c t"), in_=self.pb[bank][:, :].rearrange("p (c t) -> p c t", t=32)))
                    if not last:
                        for c16 in range(8):
                            c_abs = i * 128 + c16 * 16
                            self.ld([self.bGC[i]], [btc], tcx[:], bass.AP(tensor=self.GC, offset=c_abs * 512, ap=[[1, 128], [512, 16], [1, 384]]))
                            bank = 2 + c16 % 2
                            for cc in range(16):
                                c = c16 * 16 + cc
                                for idx, j in enumerate([1, 0, 2]):
                                    d = 1 - j
                                    s_lo, s_hi = (0, 2 - d) if d >= 0 else (-d, 2)
                                    self.T([btc, bZ], [self.bpb[bank]], lambda: nc.tensor.matmul(
                                        self.pb[bank][:, cc * 2 + s_lo + d:cc * 2 + s_hi + d], lhsT=tcx[:, cc, j * 128:(j + 1) * 128], rhs=Z[:, c, 32 + s_lo:32 + s_hi],
                                        start=(idx == 0), stop=(idx == 2)))
                            self.V([self.bpb[bank]], [bys], lambda: nc.vector.tensor_copy(
                                out=ysb[:, 32:34, c16 * 16:(c16 + 1) * 16].rearrange("p t c -> p c t"), in_=self.pb[bank][:, 0:32].rearrange("p (c t) -> p c t", t=2)))
                    for (tok0, n) in self.tok_groups(not last):
                        nt = n // 128
                        bank = 4 + (tok0 // 512) % 2
                        for j in range(nt):
                            self.T([bys, self.b_const], [self.bpb[bank]], lambda: nc.tensor.matmul(
                                self.pb[bank][:, j * 128:(j + 1) * 128], lhsT=ysb[:, tok0 // 128 + j, :], rhs=self.antib[:], start=True, stop=True))
                        ho, bho = ho_r.next()
                        self.V([self.bpb[bank], bYc], [bho], lambda: nc.vector.tensor_tensor(out=ho[:, 0:n], in0=self.pb[bank][:, 0:n], in1=Yc[:, 0, tok0:tok0 + n], op=ALU.mult))
                        self.ld([bho], [self.bMIX[2][i]], self.MIX.ap()[2, i * 128:(i + 1) * 128, tok0:tok0 + n], ho[:, 0:n])
                    self.fw.barrier()

    def phase_merge(self, l, last):
        nc = self.nc
        with ExitStack() as es:
            Wg = self.sbc(es, "mWg", [128, 8, 3072], BF16); Wb = self.sbc(es, "mWb", [128, 12, D], BF16); Wo = self.sbc(es, "mWo", [128, 8, D], BF16)
            bW = Buf()
            for cg in range(3):
                self.ldcast([], [bW], Wg[:, :, cg * 1024:(cg + 1) * 1024], self.wslice("w_in", l, C_MG + cg * 1024, 1024))
            self.ldcast([], [bW], Wb[:], self.w("w_branch").ap()[l].rearrange("(k p) n -> p k n", p=128))
            self.ldcast([], [bW], Wo[:], self.w("w_out").ap()[l].rearrange("(k p) n -> p k n", p=128))
            with ExitStack() as tmp:
                bgc, bbg = self.load_cols(es, "bgc", self.w("b_gate").ap()[l], 1, 3072, rows_es=tmp)
                self.fw.barrier()
            g1 = [self.sbc(es, f"g1_{r}", [128, D], F32) for r in range(2)]; bg1 = Buf()
            for r in range(2):
                self.ld([self.bMODV], [bg1], g1[r][:], self.bc_ap(self.MODV, r * 6 * D + 2 * D, D))
            MG = 512
            mix_r = Rot(nc, es, "mix", [128, 12, MG], BF16, 1)
            gs_r = Rot(nc, es, "gsb", [128, MG], F32, 3)
            macc = self.sbc(es, "macc", [128, MG], F32); bmacc = Buf()
            mtmp_r = Rot(nc, es, "mtmp", [128, MG], F32, 2)
            mT_r = Rot(nc, es, "mT", [128, 8, MG], BF16, 1)
            x_r = Rot(nc, es, "mx", [128, D], F32, 1)
            xo_r = Rot(nc, es, "mxo", [128, D], F32, 1)
            cnt = 0
            for (tok0, n) in self.tok_groups(not last):
                r = 0 if tok0 < L else 1
                mix, bmix = mix_r.next()
                self.ld([b for br in self.bMIX for b in br], [bmix], mix[:, :, 0:n],
                        self.MIX.ap()[:, :, tok0:tok0 + n].rearrange("b (k p) n -> p (b k) n", p=128))
                mT, bmT = mT_r.next()
                for oc in range(8):
                    for br in range(3):
                        bank_g = cnt % 3; bank_b = 3 + cnt % 3; cnt += 1
                        self.proj(bank_g, Wg, bW, br * 1024 + oc * 128, 128, tok0, n)
                        gsb, bgs = gs_r.next()
                        self.A([self.bpb[bank_g], bbg], [bgs], lambda: nc.scalar.activation(
                            out=gsb[:, 0:n], in_=self.pb[bank_g][:, 0:n], func=AF.Sigmoid, bias=bgc[:, br * 8 + oc, 0:1]))
                        for kc in range(4):
                            self.T([bW, bmix], [self.bpb[bank_b]], lambda: nc.tensor.matmul(
                                self.pb[bank_b][:, 0:n], lhsT=Wb[:, br * 4 + kc, oc * 128:(oc + 1) * 128], rhs=mix[:, br * 4 + kc, 0:n],
                                start=(kc == 0), stop=(kc == 3)))
                        if br == 0:
                            self.V([self.bpb[bank_b], bgs], [bmacc], lambda: nc.vector.tensor_tensor(out=macc[:, 0:n], in0=self.pb[bank_b][:, 0:n], in1=gsb[:, 0:n], op=ALU.mult))
                        else:
                            mt, bmt = mtmp_r.next()
                            self.V([self.bpb[bank_b], bgs], [bmt], lambda: nc.vector.tensor_tensor(out=mt[:, 0:n], in0=self.pb[bank_b][:, 0:n], in1=gsb[:, 0:n], op=ALU.mult))
                            if br == 1:
                                self.G([bmt, bmacc], [bmacc], lambda: nc.gpsimd.tensor_tensor(out=macc[:, 0:n], in0=macc[:, 0:n], in1=mt[:, 0:n], op=ALU.add))
                            else:
                                self.G([bmt, bmacc], [bmT], lambda: nc.gpsimd.tensor_tensor(out=mT[:, oc, 0:n], in0=macc[:, 0:n], in1=mt[:, 0:n], op=ALU.add))
                for j in range(n // 128):
                    t = tok0 // 128 + j
                    xt, bx = x_r.next()
                    self.ld([self.bXS[t]], [bx], xt[:], self.XS.ap()[t * 128:(t + 1) * 128, :])
                    xo, bxo = xo_r.next()
                    for half in range(2):
                        bank = 6 + half
                        for oc in range(8):
                            self.T([bmT, bW], [self.bpb[bank]], lambda: nc.tensor.matmul(
                                self.pb[bank][:, :], lhsT=mT[:, oc, j * 128:(j + 1) * 128], rhs=Wo[:, oc, half * 512:(half + 1) * 512],
                                start=(oc == 0), stop=(oc == 7)))
                        self.V([self.bpb[bank], bg1], [bxo], lambda: nc.vector.tensor_tensor(
                            out=xo[:, half * 512:(half + 1) * 512], in0=self.pb[bank][:, :], in1=g1[r][:, half * 512:(half + 1) * 512], op=ALU.mult))
                    self.G([bxo, bx], [bxo], lambda: nc.gpsimd.tensor_tensor(out=xo[:], in0=xo[:], in1=xt[:], op=ALU.add))
                    self.ld([bxo], [self.bXS[t]], self.XS.ap()[t * 128:(t + 1) * 128, :], xo[:])

    def phase_moe(self, l, last):
        nc = self.nc
        TG = 1024
        with ExitStack() as es:
            tmp = ExitStack()
            Al, Sl, bt = self.mod_tables(es, l, "norm2_g", 3 * D, 4 * D, "n2")
            g2 = [self.sbc(es, f"g2_{r}", [128, D], F32) for r in range(2)]; bg2 = Buf()
            for r in range(2):
                self.ld([self.bMODV], [bg2], g2[r][:], self.bc_ap(self.MODV, r * 6 * D + 5 * D, D))
            fng = None
            if last:
                fng = self.sbc(es, "fng", [128, D], F32)
                self.ld([], [bg2], fng[:], self.bc_ap(self.w("final_norm_g"), 0, D))
            b1c, bb1 = self.load_cols(es, "b1c", self.w("moe_b1").ap()[l], 32, 2048, bank=7, rows_es=tmp)
            self.fw.barrier()
            tmp.close()
            self.V([bb1], [bb1], lambda: nc.vector.tensor_scalar(out=b1c[:, 8:16, :], in0=b1c[:, 8:16, :], scalar1=1.0, scalar2=None, op0=ALU.add))
            b2s = self.sbc(es, "b2s", [32, D], F32); bb2 = Buf()
            self.ld([], [bb2], b2s[:], self.w("moe_b2").ap()[l])
            Wr = self.sbc(es, "Wr", [128, 8, NEXP], F32); bWr = Buf()
            self.ld([], [bWr], Wr[:], self.w("router_w").ap()[l].rearrange("(k p) n -> p k n", p=128))
            rb = self.sbc(es, "rb", [128, NEXP], F32)
            self.ld([], [bWr], rb[:], self.bc_ap(self.w("router_b"), l * NEXP, NEXP))
            _h2 = self.sbc(es, "h2T", [128, 8, TG], BF16); _bh2 = Buf()
            h2Ts = [_h2, _h2]; bh2s = [_bh2, _bh2]
            h32_r = Rot(nc, es, "h32", [128, 8, 128], F32, 1)
            _cb = self.sbc(es, "comb", [128, 8, NEXP], F32)
            _cbs = self.sbc(es, "combs", [128, 8, NEXP], F32)
            _bcb = Buf()
            combA = [_cb, _cb]; combsA = [_cbs, _cbs]; bcombA = [_bcb, _bcb]
            combT = self.sbc(es, "combT", [32, 8, 128], F32); bcT = Buf()
            yacc = self.sbc(es, "yacc", [128, 8, D], F32); byacc = [Buf() for _ in range(8)]
            actT = self.sbc(es, "actT", [128, 8, TG], BF16); bact = [Buf() for _ in range(2)]
            W1r = Rot(nc, es, "W1", [128, 8, 2 * DFF], BF16, 1)
            W2r = Rot(nc, es, "W2", [128, 8, D], BF16, 2)
            xr = Rot(nc, es, "ex", [128, D], F32, 1)
            xnr = Rot(nc, es, "exn", [128, D], F32, 2)
            junk = self.sbc(es, "ejunk", [128, D], F32); bj = Buf()
            str_ = Rot(nc, es, "est", [128, 4], F32, 2)
            lg_r = Rot(nc, es, "elg", [128, 48], F32, 2)
            g_r = Rot(nc, es, "eg", [128, 512], F32, 2)
            s_r = Rot(nc, es, "es", [128, 512], BF16, 2)
            l_r = Rot(nc, es, "el", [128, 512], F32, 2)
            groups = [(i * TG, TG) for i in range(L // TG)] + ([] if last else [(L, LC)])
            state = {}

            def pro1(gi, j):
                tok0, n = groups[gi]
                r = 0 if tok0 < L else 1
                t = tok0 // 128 + j
                xt, bx = xr.next(); xn, bxn = xnr.next(); st, bst = str_.next()
                self.ld([self.bXS[t]], [bx], xt[:], self.XS.ap()[t * 128:(t + 1) * 128, :])
                self.norm_tile(xt[:], bx, xn[:], bxn, Al[r], Sl[r], bt, (junk, bj, st, bst), add_on_dve=True)
                state[("xn", j)] = (xn, bxn)

            def pro2(gi, j):
                pg = gi % 2
                h2T, bh2 = h2Ts[pg], bh2s[pg]
                xn, bxn = state[("xn", j)]
                h32, bh32 = h32_r.next()
                state["h32"] = (h32, bh32)
                for half in range(2):
                    bank = half
                    for kk in range(4):
                        k = half * 4 + kk
                        self.T([bxn, self.b_const], [self.bpb[bank]], lambda: nc.tensor.transpose(
                            out=self.pb[bank][:, kk * 128:(kk + 1) * 128], in_=xn[:, k * 128:(k + 1) * 128], identity=self.ident32[:]))
                    src = self.pb[bank][:, :].rearrange("p (a b) -> p a b", a=4)
                    self.A([self.bpb[bank]], [bh2], lambda: nc.scalar.copy(out=h2T[:, half * 4:half * 4 + 4, j * 128:(j + 1) * 128], in_=src))
                    self.V([self.bpb[bank]], [bh32], lambda: nc.vector.tensor_copy(out=h32[:, half * 4:half * 4 + 4, :], in_=src))

            def pro3(gi, j):
                pg = gi % 2
                comb, combs, bcomb = combA[pg], combsA[pg], bcombA[pg]
                h32, bh32 = state["h32"]
                for k in range(8):
                    self.T([bh32, bWr], [self.bpb[2]], lambda: nc.tensor.matmul(
                        self.pb[2][:, 0:NEXP], lhsT=h32[:, k, :], rhs=Wr[:, k, :], start=(k == 0), stop=(k == 7)))
                lg, blg = lg_r.next()
                self.V([self.bpb[2], bWr], [blg], lambda: nc.vector.tensor_tensor(out=lg[:, 0:32], in0=self.pb[2][:, 0:NEXP], in1=rb[:], op=ALU.add))
                self.V([blg], [blg], lambda: nc.vector.max(out=lg[:, 32:40], in_=lg[:, 0:32]))
                self.V([blg], [blg], lambda: nc.vector.tensor_scalar(out=lg[:, 40:41], in0=lg[:, 32:33], scalar1=-1.0, scalar2=None, op0=ALU.mult))
                self.V([blg], [bcomb], lambda: nc.vector.tensor_scalar(out=combs[:, j, :], in0=lg[:, 0:32], scalar1=lg[:, 35:36], scalar2=None, op0=ALU.is_ge))
                self.A([blg], [blg], lambda: nc.scalar.activation(out=lg[:, 0:32], in_=lg[:, 0:32], func=AF.Exp, bias=lg[:, 40:41]))
                self.V([blg, bcomb], [bcomb], lambda: nc.vector.tensor_tensor(out=combs[:, j, :], in0=combs[:, j, :], in1=lg[:, 0:32], op=ALU.mult))
                self.V([bcomb], [blg], lambda: nc.vector.reduce_sum(out=lg[:, 41:42], in_=combs[:, j, :], axis=mybir.AxisListType.X))
                self.V([blg], [blg], lambda: nc.vector.reciprocal(out=lg[:, 42:43], in_=lg[:, 41:42]))
                self.V([blg, bcomb], [bcomb], lambda: nc.vector.tensor_scalar(out=comb[:, j, :], in0=combs[:, j, :], scalar1=lg[:, 42:43], scalar2=None, op0=ALU.mult))
                self.V([bcomb], [bcomb], lambda: nc.vector.tensor_scalar(out=combs[:, j, :], in0=comb[:, j, :], scalar1=1.0 / 1.702, scalar2=None, op0=ALU.mult))

            def bias_init(gi, j0=None):
                tok0, n = groups[gi]
                pg = gi % 2
                comb, bcomb = combA[pg], bcombA[pg]
                for j in (range(n // 128) if j0 is None else [j0]):
                    self.T([bcomb, self.b_const], [self.bpb[3]], lambda: nc.tensor.transpose(out=self.pb[3][0:32, 0:128], in_=comb[:, j, :], identity=self.ident32[:]))
                    self.V([self.bpb[3]], [bcT], lambda: nc.vector.tensor_copy(out=combT[:, j, :], in_=self.pb[3][0:32, 0:128]))
                    for half in range(2):
                        bank = 4 + half
                        self.T([bcT, bb2], [self.bpb[bank]], lambda: nc.tensor.matmul(
                            self.pb[bank][:, :], lhsT=combT[:, j, :], rhs=b2s[:, half * 512:(half + 1) * 512], start=True, stop=True))
                        self.A([self.bpb[bank]], [byacc[j]], lambda: nc.scalar.copy(out=yacc[:, j, half * 512:(half + 1) * 512], in_=self.pb[bank][:, :]))

            def experts(gi, hooks):
                tok0, n = groups[gi]
                pg = gi % 2
                h2T, bh2 = h2Ts[pg], bh2s[pg]
                combs, bcomb = combsA[pg], bcombA[pg]
                ntile = n // 128
                ntg = (n + 511) // 512
                for e in range(NEXP):
                    W1, bW1 = W1r.next(); W2, bW2 = W2r.next()
                    self.ldcast([], [bW1], W1[:], self.w("moe_w1").ap()[l, e].rearrange("(k p) n -> p k n", p=128))
                    self.ldcast([], [bW2], W2[:], self.w("moe_w2").ap()[l, e].rearrange("(k p) n -> p k n", p=128))
                    cnt = 0
                    for tg in range(ntg):
                        t0 = tg * 512; m = min(512, n - t0)
                        for fc in range(8):
                            bg_, bl_ = (0, 1) if cnt % 2 == 0 else (2, 3)
                            cnt += 1
                            for k in range(8):
                                self.T([bW1, bh2], [self.bpb[bg_]], lambda: nc.tensor.matmul(
                                    self.pb[bg_][:, 0:m], lhsT=W1[:, k, fc * 128:(fc + 1) * 128], rhs=h2T[:, k, t0:t0 + m], start=(k == 0), stop=(k == 7)))
                            for k in range(8):
                                self.T([bW1, bh2], [self.bpb[bl_]], lambda: nc.tensor.matmul(
                                    self.pb[bl_][:, 0:m], lhsT=W1[:, k, DFF + fc * 128:DFF + (fc + 1) * 128], rhs=h2T[:, k, t0:t0 + m], start=(k == 0), stop=(k == 7)))
                            gg, bgg = g_r.next(); ss, bss = s_r.next(); ll, bll = l_r.next()
                            self.V([self.bpb[bg_], bb1], [bgg], lambda: nc.vector.tensor_scalar(
                                out=gg[:, 0:m], in0=self.pb[bg_][:, 0:m], scalar1=b1c[:, fc, e:e + 1], scalar2=7.0, op0=ALU.add, op1=ALU.min))
                            self.A([bgg], [bss], lambda: nc.scalar.activation(out=ss[:, 0:m], in_=gg[:, 0:m], func=AF.Silu, scale=1.702))
                            self.V([self.bpb[bl_], bb1], [bll], lambda: nc.vector.tensor_scalar(
                                out=ll[:, 0:m], in0=self.pb[bl_][:, 0:m], scalar1=b1c[:, 8 + fc, e:e + 1], scalar2=8.0, op0=ALU.add, op1=ALU.min))
                            self.V([bll, bss], [bact[tg]], lambda: nc.vector.scalar_tensor_tensor(
                                out=actT[:, fc, t0:t0 + m], in0=ll[:, 0:m], scalar=-6.0, in1=ss[:, 0:m], op0=ALU.max, op1=ALU.mult))
                    for j in range(ntile):
                        for half in range(2):
                            bank = 4 + (2 * j + half) % 4
                            for fc in range(8):
                                self.T([bact[j // 4], bW2], [self.bpb[bank]], lambda: nc.tensor.matmul(
                                    self.pb[bank][:, :], lhsT=actT[:, fc, j * 128:(j + 1) * 128], rhs=W2[:, fc, half * 512:(half + 1) * 512],
                                    start=(fc == 0), stop=(fc == 7)))
                            ya = yacc[:, j, half * 512:(half + 1) * 512]
                            self.V([self.bpb[bank], bcomb, byacc[j]], [byacc[j]], lambda: nc.vector.scalar_tensor_tensor(
                                out=ya, in0=self.pb[bank][:, :], scalar=combs[:, j, e:e + 1], in1=ya, op0=ALU.mult, op1=ALU.add))
                    for fn in hooks.get(e, []):
                        fn()

            def epilogue(gi):
                tok0, n = groups[gi]
                r = 0 if tok0 < L else 1
                for j in range(n // 128):
                    t = tok0 // 128 + j
                    xt, bx = xr.next(); xn, bxn = xnr.next()
                    self.ld([self.bXS[t]], [bx], xt[:], self.XS.ap()[t * 128:(t + 1) * 128, :])
                    self.V([byacc[j], bg2], [byacc[j]], lambda: nc.vector.tensor_tensor(out=yacc[:, j, :], in0=yacc[:, j, :], in1=g2[r][:], op=ALU.mult))
                    self.V([byacc[j], bx], [bxn], lambda: nc.vector.tensor_tensor(out=xn[:], in0=yacc[:, j, :], in1=xt[:], op=ALU.add))
                    if not last:
                        self.ld([bxn], [self.bXS[t]], self.XS.ap()[t * 128:(t + 1) * 128, :], xn[:])
                    else:
                        st, bst = str_.next()
                        self.V([], [bst], lambda: nc.vector.memset(st[:, 0:1], 0.0))
                        self.A([bxn], [bj, bst], lambda: nc.scalar.activation(out=junk[:], in_=xn[:], func=AF.Square, accum_out=st[:, 0:1]))
                        self.V([bst], [bst], lambda: nc.vector.tensor_scalar(out=st[:, 1:2], in0=st[:, 0:1], scalar1=1.0 / D, scalar2=EPS, op0=ALU.mult, op1=ALU.add))
                        self.A([bst], [bst], lambda: nc.scalar.activation(out=st[:, 2:3], in_=st[:, 1:2], func=AF.Sqrt))
                        self.V([bst], [bst], lambda: nc.vector.reciprocal(out=st[:, 3:4], in_=st[:, 2:3]))
                        self.V([bxn, bst, bg2], [bx], lambda: nc.vector.scalar_tensor_tensor(
                            out=xt[:], in0=xn[:], scalar=st[:, 3:4], in1=fng[:], op0=ALU.mult, op1=ALU.mult))
                        self.ld([bx], [self.bOUT], self.OUT.ap()[t * 128:(t + 1) * 128, :], xt[:])

            for gi in range(len(groups)):
                nt = groups[gi][1] // 128
                pro1(gi, 0)
                for j in range(nt):
                    if j + 1 < nt:
                        pro1(gi, j + 1)
                    pro2(gi, j)
                    pro3(gi, j)
                    bias_init(gi, j)
                experts(gi, {})
                epilogue(gi)


def make_in_maps(prog, inputs):
    consts = host_consts()
    n = 8
    shared = {}
    reshapes = dict(ret_decay_logit=(DEPTH, 8), hy_conv_b=(DEPTH, 1, 1536), hy_b1=(DEPTH, 1, 64), hy_freq1=(DEPTH, 1, 64),
                    hy_b2=(DEPTH, 1, 64), hy_freq2=(DEPTH, 1, 64), hy_skip=(DEPTH, 1, 512), w_branch=(DEPTH, 1536, D),
                    b_gate=(DEPTH, 1, 3072), final_norm_g=(1, D))
    for name in prog.din:
        if name.startswith("k_"):
            shared[name] = np.ascontiguousarray(consts[name[2:]])
        elif name in ("x", "ctx", "cc"):
            continue
        else:
            a = np.asarray(inputs[name], dtype=np.float32)
            if name in reshapes:
                a = a.reshape(reshapes[name])
            shared[name] = np.ascontiguousarray(a)
    maps = []
    for b in range(n):
        m = dict(shared)
        if "x" in prog.din:
            m["x"] = np.ascontiguousarray(inputs["x"][b], dtype=np.float32)
        if "ctx" in prog.din:
            m["ctx"] = np.ascontiguousarray(inputs["ctx"][b], dtype=np.float32)
        if "cc" in prog.din:
            m["cc"] = np.ascontiguousarray(np.concatenate([np.asarray(inputs["c"][b], np.float32).reshape(8, 128),
                                                           np.asarray(inputs["c_ctx"], np.float32).reshape(8, 128)], 0))
        maps.append(m)
    return maps


def kernel(**inputs):
    prog = Prog()
    nc = prog.build()
    maps = make_in_maps(prog, inputs)
    res = run_bass_kernel_spmd(nc, maps, core_ids=list(range(8)))
    return np.stack([np.asarray(r["OUT"], dtype=np.float32) for r in res.results], 0)
```
